# Optimizing a Trainium2 kernel written in Bass

```python
import math
import jax, jax.numpy as jnp
from jax import lax
import numpy as np

D_MODEL = 1024
BATCH = 4
SEQ = 8192
DEPTH = 1

GLA_HEADS = 4
GLA_DK = 64
GLA_DV = 128
GLA_LOWRANK = 16
GLA_TAU = 16.0
GLA_CHUNK = 64
MOBA_HEADS = 8
MOBA_DH = 64
MOBA_BLOCK = 256
MOBA_TOPK = 3
MOBA_QCHUNK = 128
REL_BUCKETS = 32
REL_MAX_DIST = 128
MEM_LEN = 256
MEM_HEADS = 4
MEM_DH = 128
N_GROUPS = 4
EXPERTS_PER_GROUP = 8
N_EXPERTS = N_GROUPS * EXPERTS_PER_GROUP
TOPK_IN_GROUP = 2
D_EXPERT = 512
MOE_BLOCK = 128
EPS = 1e-6

GLA_QK = GLA_HEADS * GLA_DK
GLA_V = GLA_HEADS * GLA_DV
MOBA_W = MOBA_HEADS * MOBA_DH
MEM_W = MEM_HEADS * MEM_DH
IN_SPLITS = (GLA_QK, GLA_QK, GLA_V, GLA_V, GLA_LOWRANK, MOBA_W, MOBA_W, MOBA_W, D_MODEL, D_MODEL)
D_IN = sum(IN_SPLITS)
SPLIT_POINTS = tuple(sum(IN_SPLITS[:i + 1]) for i in range(len(IN_SPLITS) - 1))

kernel_name = 'hybrid_gla_moba_hmoe_block'


def rmsnorm(x, g):
    xf = x.astype(jnp.float32)
    y = xf * lax.rsqrt(jnp.mean(xf * xf, axis=-1, keepdims=True) + EPS)
    return (y * g.astype(jnp.float32)).astype(x.dtype)


def gla_chunked(q, k, v, log_a):
    B, S = q.shape[0], q.shape[1]
    C = GLA_CHUNK
    N = S // C

    def chunks(t):
        return t.astype(jnp.float32).reshape(B, N, C, GLA_HEADS, t.shape[-1]).transpose(0, 3, 1, 2, 4)

    q, k, v, log_a = chunks(q) * (GLA_DK ** -0.5), chunks(k), chunks(v), chunks(log_a)
    b = jnp.cumsum(log_a, axis=3)
    b_last = b[:, :, :, C - 1:]
    b_mid = b[:, :, :, C // 2 - 1:C // 2]
    att = jnp.einsum('bhncd,bhnsd->bhncs', q * jnp.exp(b - b_mid), k * jnp.exp(b_mid - b))
    causal = jnp.tril(jnp.ones((C, C), dtype=bool))
    att = jnp.where(causal, att, 0.0)
    o_intra = jnp.einsum('bhncs,bhnsv->bhncv', att, v)
    kv = jnp.einsum('bhncd,bhncv->bhndv', k * jnp.exp(b_last - b), v)
    decay = jnp.exp(b_last[:, :, :, 0])

    def step(state, inp):
        dec, kv_c = inp
        return dec[..., None] * state + kv_c, state

    init = jnp.zeros((B, GLA_HEADS, GLA_DK, GLA_DV), jnp.float32)
    _, states = lax.scan(step, init, (jnp.moveaxis(decay, 2, 0), jnp.moveaxis(kv, 2, 0)))
    states = jnp.moveaxis(states, 0, 2)
    o_inter = jnp.einsum('bhncd,bhndv->bhncv', q * jnp.exp(b), states)
    o = o_intra + o_inter
    return o.transpose(0, 2, 3, 1, 4).reshape(B, S, GLA_HEADS, GLA_DV)


def t5_bucket(dist):
    n = jnp.maximum(dist, 0)
    max_exact = REL_BUCKETS // 2
    nf = jnp.maximum(n, 1).astype(jnp.float32)
    large = max_exact + (jnp.log(nf / max_exact) / math.log(REL_MAX_DIST / max_exact)
                         * (REL_BUCKETS - max_exact)).astype(jnp.int32)
    large = jnp.minimum(large, REL_BUCKETS - 1)
    return jnp.where(n < max_exact, n, large)


def moba_attention(q, k, v, rel_bias):
    B, S = q.shape[0], q.shape[1]
    H, BS, QC = MOBA_HEADS, MOBA_BLOCK, MOBA_QCHUNK
    Sp = -(-S // BS) * BS
    NB = Sp // BS
    NC = Sp // QC
    K = min(MOBA_TOPK, NB)

    def heads(t):
        t = t.reshape(B, S, H, MOBA_DH).transpose(0, 2, 1, 3)
        return jnp.pad(t, ((0, 0), (0, 0), (0, Sp - S), (0, 0)))

    q = heads(q) * (MOBA_DH ** -0.5)
    kb = heads(k).reshape(B, H, NB, BS, MOBA_DH)
    vb = heads(v).reshape(B, H, NB, BS, MOBA_DH)
    k_mean = jnp.mean(kb.astype(jnp.float32), axis=3)
    gate = jnp.einsum('bhsd,bhnd->bhsn', q.astype(jnp.float32), k_mean)
    q_blk = jnp.arange(Sp) // BS
    fully_past = jnp.arange(NB)[None, :] < q_blk[:, None]
    gate = jnp.where(fully_past, gate, -jnp.inf)
    _, sel = lax.top_k(gate, K)
    sel_valid = sel < q_blk[:, None]

    def to_chunks(t):
        return jnp.moveaxis(t.reshape(B, H, NC, QC, t.shape[-1]), 2, 0)

    bias_hb = rel_bias.T.astype(jnp.float32)
    b_idx = jnp.arange(B)[:, None, None, None]
    h_idx = jnp.arange(H)[None, :, None, None]
    offs = jnp.arange(BS)

    def one_chunk(args):
        c, qc, selc, validc = args
        q_pos = c * QC + jnp.arange(QC)
        own = (c * QC) // BS
        k_own = lax.dynamic_index_in_dim(kb, own, axis=2, keepdims=False)
        v_own = lax.dynamic_index_in_dim(vb, own, axis=2, keepdims=False)
        d_own = q_pos[:, None] - (own * BS + offs)[None, :]
        s_own = jnp.einsum('bhqd,bhkd->bhqk', qc, k_own).astype(jnp.float32) + bias_hb[:, t5_bucket(d_own)]
        s_own = jnp.where(d_own >= 0, s_own, -jnp.inf)
        k_sel = kb[b_idx, h_idx, selc]
        v_sel = vb[b_idx, h_idx, selc]
        d_sel = q_pos[:, None, None] - (selc[..., None] * BS + offs)
        s_sel = (jnp.einsum('bhqd,bhqjkd->bhqjk', qc, k_sel).astype(jnp.float32)
                 + bias_hb[h_idx[..., None], t5_bucket(d_sel)])
        s_sel = jnp.where(validc[..., None], s_sel, -jnp.inf)
        logits = jnp.concatenate([s_own, s_sel.reshape(B, H, QC, K * BS)], axis=-1)
        p = jax.nn.softmax(logits, axis=-1)
        p_own = p[..., :BS].astype(v_own.dtype)
        p_sel = p[..., BS:].reshape(B, H, QC, K, BS).astype(v_sel.dtype)
        return (jnp.einsum('bhqk,bhkd->bhqd', p_own, v_own)
                + jnp.einsum('bhqjk,bhqjkd->bhqd', p_sel, v_sel))

    out = lax.map(one_chunk, (jnp.arange(NC), to_chunks(q), to_chunks(sel), to_chunks(sel_valid)))
    out = out.transpose(1, 0, 3, 2, 4).reshape(B, Sp, H * MOBA_DH)
    return out[:, :S]


def mixer(h, rel_bias, w_in, w_alpha_up, b_alpha, g_gla_head, w_proj_gla, w_proj_moba, w_out):
    B, S, _ = h.shape
    proj = h @ w_in
    q_g, k_g, v_g, r_g, a_lr, q_m, k_m, v_m, z_g, z_m = jnp.split(proj, SPLIT_POINTS, axis=-1)
    log_a = jax.nn.log_sigmoid((a_lr @ w_alpha_up + b_alpha).astype(jnp.float32)) / GLA_TAU
    o_g = gla_chunked(q_g.reshape(B, S, GLA_HEADS, GLA_DK), k_g.reshape(B, S, GLA_HEADS, GLA_DK),
                      v_g.reshape(B, S, GLA_HEADS, GLA_DV), log_a.reshape(B, S, GLA_HEADS, GLA_DK))
    o_g = rmsnorm(o_g, g_gla_head).reshape(B, S, GLA_V).astype(h.dtype) * jax.nn.silu(r_g)
    o_m = moba_attention(q_m, k_m, v_m, rel_bias)
    merged = jax.nn.sigmoid(z_g) * (o_g @ w_proj_gla) + jax.nn.sigmoid(z_m) * (o_m @ w_proj_moba)
    return merged @ w_out


def memory_attention(h, mem_n, w_cq, w_ckv, w_co):
    B, S, _ = h.shape
    M = mem_n.shape[1]
    q = (h @ w_cq).reshape(B, S, MEM_HEADS, MEM_DH)
    k, v = jnp.split((mem_n @ w_ckv).reshape(B, M, 2, MEM_HEADS, MEM_DH), 2, axis=2)
    k, v = k[:, :, 0], v[:, :, 0]
    s = jnp.einsum('bqhd,bkhd->bhqk', q, k).astype(jnp.float32) * (MEM_DH ** -0.5)
    p = jax.nn.softmax(s, axis=-1).astype(v.dtype)
    o = jnp.einsum('bhqk,bkhd->bqhd', p, v).reshape(B, S, MEM_W)
    return o @ w_co


def hier_moe(h, w_rg, b_rg, w_re, b_re, w_gate, w_up, w_down):
    B, S, D = h.shape
    T = B * S
    xt = h.reshape(T, D)
    g_prob = jax.nn.softmax((xt @ w_rg).astype(jnp.float32) + b_rg, axis=-1)
    p_grp, grp = lax.top_k(g_prob, 1)
    e_logits = ((xt @ w_re).astype(jnp.float32) + b_re).reshape(T, N_GROUPS, EXPERTS_PER_GROUP)
    e_in = jnp.take_along_axis(e_logits, grp[:, :, None], axis=1)[:, 0]
    top_p, top_i = lax.top_k(jax.nn.softmax(e_in, axis=-1), TOPK_IN_GROUP)
    weights = p_grp * top_p / jnp.sum(top_p, axis=-1, keepdims=True)
    expert = grp * EXPERTS_PER_GROUP + top_i
    A = T * TOPK_IN_GROUP
    flat_e = expert.reshape(A)
    flat_t = jnp.repeat(jnp.arange(T, dtype=jnp.int32), TOPK_IN_GROUP)
    flat_w = weights.reshape(A)
    order = jnp.argsort(flat_e)
    se = flat_e[order]
    counts = jnp.zeros((N_EXPERTS,), jnp.int32).at[flat_e].add(1)
    padded = (counts + MOE_BLOCK - 1) // MOE_BLOCK * MOE_BLOCK
    start = jnp.cumsum(counts) - counts
    pend = jnp.cumsum(padded)
    pstart = pend - padded
    dest = pstart[se] + (jnp.arange(A) - start[se])
    cap = (A + N_EXPERTS * (MOE_BLOCK - 1) + MOE_BLOCK - 1) // MOE_BLOCK * MOE_BLOCK
    n_blk = cap // MOE_BLOCK
    buf_t = jnp.zeros((cap,), jnp.int32).at[dest].set(flat_t[order])
    buf_w = jnp.zeros((cap,), jnp.float32).at[dest].set(flat_w[order])
    blk_e = jnp.minimum(jnp.searchsorted(pend, jnp.arange(n_blk) * MOE_BLOCK, side='right'), N_EXPERTS - 1)

    def run_block(args):
        e, tok, w = args
        xb = xt[tok]
        hid = jax.nn.silu(xb @ w_gate[e]) * (xb @ w_up[e])
        return (hid @ w_down[e]) * w[:, None].astype(xb.dtype)

    yb = lax.map(run_block, (blk_e, buf_t.reshape(n_blk, MOE_BLOCK), buf_w.reshape(n_blk, MOE_BLOCK)))
    out = jnp.zeros((T, D), h.dtype).at[buf_t].add(yb.reshape(cap, D).astype(h.dtype))
    return out.reshape(B, S, D)


def setup_inputs(seed: int = 0) -> dict:
    key = jax.random.key(seed)
    ks = jax.random.split(key, 26)
    f32 = jnp.float32
    L = DEPTH

    def nrm(k, shape, scale):
        return jax.random.normal(k, shape, f32) * scale

    def gain(k, shape):
        return 1.0 + 0.02 * jax.random.normal(k, shape, f32)

    return {
        'x': nrm(ks[0], (BATCH, SEQ, D_MODEL), 1.0),
        'mem': nrm(ks[1], (BATCH, MEM_LEN, D_MODEL), 1.0),
        'g_mem': gain(ks[2], (D_MODEL,)),
        'rel_bias': nrm(ks[3], (REL_BUCKETS, MOBA_HEADS), 0.5),
        'g_mix': gain(ks[4], (L, D_MODEL)),
        'w_in': nrm(ks[5], (L, D_MODEL, D_IN), D_MODEL ** -0.5),
        'w_alpha_up': nrm(ks[6], (L, GLA_LOWRANK, GLA_QK), GLA_LOWRANK ** -0.5),
        'b_alpha': nrm(ks[7], (L, GLA_QK), 0.1),
        'g_gla_head': gain(ks[8], (L, GLA_HEADS, GLA_DV)),
        'w_proj_gla': nrm(ks[9], (L, GLA_V, D_MODEL), GLA_V ** -0.5),
        'w_proj_moba': nrm(ks[10], (L, MOBA_W, D_MODEL), MOBA_W ** -0.5),
        'w_out': nrm(ks[11], (L, D_MODEL, D_MODEL), D_MODEL ** -0.5),
        'g_cross': gain(ks[12], (L, D_MODEL)),
        'w_cq': nrm(ks[13], (L, D_MODEL, MEM_W), D_MODEL ** -0.5),
        'w_ckv': nrm(ks[14], (L, D_MODEL, 2 * MEM_W), D_MODEL ** -0.5),
        'w_co': nrm(ks[15], (L, MEM_W, D_MODEL), MEM_W ** -0.5),
        'g_moe': gain(ks[16], (L, D_MODEL)),
        'w_router_group': nrm(ks[17], (L, D_MODEL, N_GROUPS), D_MODEL ** -0.5),
        'b_router_group': nrm(ks[18], (L, N_GROUPS), 0.01),
        'w_router_expert': nrm(ks[19], (L, D_MODEL, N_EXPERTS), D_MODEL ** -0.5),
        'b_router_expert': nrm(ks[20], (L, N_EXPERTS), 0.01),
        'w_exp_gate': nrm(ks[21], (L, N_EXPERTS, D_MODEL, D_EXPERT), D_MODEL ** -0.5),
        'w_exp_up': nrm(ks[22], (L, N_EXPERTS, D_MODEL, D_EXPERT), D_MODEL ** -0.5),
        'w_exp_down': nrm(ks[23], (L, N_EXPERTS, D_EXPERT, D_MODEL), D_EXPERT ** -0.5),
        'g_final': gain(ks[24], (D_MODEL,)),
    }


def reference(x, mem, g_mem, rel_bias, g_mix, w_in, w_alpha_up, b_alpha, g_gla_head, w_proj_gla,
              w_proj_moba, w_out, g_cross, w_cq, w_ckv, w_co, g_moe, w_router_group, b_router_group,
              w_router_expert, b_router_expert, w_exp_gate, w_exp_up, w_exp_down, g_final):
    mem_n = rmsnorm(mem, g_mem)
    for l in range(DEPTH):
        x = x + mixer(rmsnorm(x, g_mix[l]), rel_bias, w_in[l], w_alpha_up[l], b_alpha[l], g_gla_head[l],
                      w_proj_gla[l], w_proj_moba[l], w_out[l])
        x = x + memory_attention(rmsnorm(x, g_cross[l]), mem_n, w_cq[l], w_ckv[l], w_co[l])
        x = x + hier_moe(rmsnorm(x, g_moe[l]), w_router_group[l], b_router_group[l], w_router_expert[l],
                         b_router_expert[l], w_exp_gate[l], w_exp_up[l], w_exp_down[l])
    return rmsnorm(x, g_final)
```

```python
import numpy as np
import ml_dtypes
from contextlib import ExitStack
import concourse.bass as bass
import concourse.mybir as mybir
from concourse.bass_utils import run_bass_kernel_spmd

F32 = mybir.dt.float32
BF16 = mybir.dt.bfloat16
I32 = mybir.dt.int32
AF = mybir.ActivationFunctionType
ALU = mybir.AluOpType
AX = mybir.AxisListType

NCORES = 8
D = 1024
TOWN = 4096
TEXT = 8192
DIN = 5136
CAP = 384
NEXP = 32
NROWS = NEXP * CAP
EPS = 1e-6
NEG = -30000.0

C_QG, C_KG, C_VG, C_RG, C_A, C_QM, C_KM, C_VM, C_ZG, C_ZM = 0, 256, 512, 1024, 1536, 1552, 2064, 2576, 3088, 4112


class Prog:
    ENGS = ["pe", "act", "dve", "pool", "sp"]

    def __init__(self, nc, es):
        self.nc, self.es = nc, es
        self.q = {e: [] for e in self.ENGS}
        self.seq = {e: 0 for e in self.ENGS}
        self.cnt = {e: 0 for e in self.ENGS}
        self.sem = {}
        self.nsem = 0
        for e in self.ENGS:
            self._new_epoch(e)
        self.dma_pool = [es.enter_context(nc.semaphore(f"dq{i}")) for i in range(56)]
        self.dma_cnt = [0] * len(self.dma_pool)
        self.dma_key = {}
        self.dma_next = 0
        self.dma_nextq = [0, 0]
        self.last_w = {}
        self.readers = {}
        self.waited = {e: {} for e in self.ENGS}
        self.out_tokens = []

    def _new_epoch(self, e):
        self.sem[e] = self.es.enter_context(self.nc.semaphore(f"e_{e}_{self.nsem}"))
        self.nsem += 1
        self.cnt[e] = 0

    def _collect(self, reads, writes):
        deps = []
        for k in reads:
            t = self.last_w.get(k)
            if t is not None:
                deps.append(t)
        for k in writes:
            t = self.last_w.get(k)
            if t is not None:
                deps.append(t)
            deps.extend(self.readers.get(k, ()))
        return deps

    def _waits(self, eng, deps):
        best = {}
        for (sem, val, teng, tseq) in deps:
            if teng == eng and eng == "pe":
                continue
            key = id(sem)
            if self.waited[eng].get(key, 0) >= val:
                continue
            if key not in best or best[key][1] < val:
                best[key] = (sem, val)
        for key, (sem, val) in best.items():
            self.waited[eng][key] = val
        return list(best.values())

    def _record(self, tok, reads, writes):
        for k in writes:
            self.last_w[k] = tok
            self.readers[k] = []
        for k in reads:
            if k in writes:
                continue
            self.readers.setdefault(k, []).append(tok)

    def op(self, eng, fn, r=(), w=()):
        r, w = list(r), list(w)
        waits = self._waits(eng, self._collect(r, w))
        if self.cnt[eng] >= 20000:
            self._new_epoch(eng)
        self.cnt[eng] += 1
        self.seq[eng] += 1
        sem, val = self.sem[eng], self.cnt[eng]

        def emit(e, fn=fn, waits=waits, sem=sem):
            for (s, v) in waits:
                e.wait_ge(s, v)
            fn(e).then_inc(sem, 1)
        self.q[eng].append(emit)
        tok = (sem, val, eng, self.seq[eng])
        self._record(tok, r, w)
        return tok

    def dma(self, queue, fn, semkey, r=(), w=(), is_out=False):
        r, w = list(r), list(w)
        waits = self._waits(queue, self._collect(r, w))
        if semkey not in self.dma_key:
            half = len(self.dma_pool) // 2
            qi = 0 if queue == "pool" else 1
            self.dma_key[semkey] = (qi * half + self.dma_nextq[qi] % half, queue)
            self.dma_nextq[qi] += 1
        si, q0 = self.dma_key[semkey]
        assert q0 == queue, (semkey, q0, queue)
        self.dma_cnt[si] += 16
        sem, val = self.dma_pool[si], self.dma_cnt[si]

        def emit(e, fn=fn, waits=waits, sem=sem):
            for (s, v) in waits:
                e.wait_ge(s, v)
            fn(e).then_inc(sem, 16)
        self.q[queue].append(emit)
        tok = (sem, val, "dma", 0)
        self._record(tok, r, w)
        if is_out:
            self.out_tokens.append(tok)
        return tok

    def barrier(self):
        toks = []
        for t in self.last_w.values():
            toks.append(t)
        for l in self.readers.values():
            toks.extend(l)
        for e in self.ENGS:
            waits = self._waits(e, [(s, v, "x", 0) for (s, v, _, _) in toks])
            if waits:
                def emit(en, waits=waits):
                    for (s, v) in waits:
                        en.wait_ge(s, v)
                self.q[e].append(emit)
        self.last_w.clear()
        self.readers.clear()
        self.dma_key.clear()

    def finish(self):
        self.barrier()
        with self.nc.Block() as block:
            @block.tensor
            def _(e):
                for f in self.q["pe"]:
                    f(e)

            @block.scalar
            def _(e):
                for f in self.q["act"]:
                    f(e)

            @block.vector
            def _(e):
                for f in self.q["dve"]:
                    f(e)

            @block.gpsimd
            def _(e):
                for f in self.q["pool"]:
                    f(e)

            @block.sync
            def _(e):
                for f in self.q["sp"]:
                    f(e)

    def mm(self, out, lhsT, rhs, start, stop, r, w):
        return self.op("pe", lambda e: e.matmul(out, lhsT, rhs, start=start, stop=stop), r, w)

    def tr(self, out, in_, ident, r, w):
        return self.op("pe", lambda e: e.transpose(out, in_, ident), r, w)

    def act(self, eng, out, in_, func, r, w, bias=None, scale=1.0, accum=None):
        def fn(e):
            kw = {}
            if bias is not None:
                kw["bias"] = bias
            if accum is not None:
                kw["accum_out"] = accum
            return e.activation(out, in_, func, scale=scale, **kw)
        return self.op(eng, fn, r, w)

    def ts(self, eng, out, in0, s1, s2, op0, op1, r, w):
        if op1 is None:
            return self.op(eng, lambda e: e.tensor_scalar(out, in0, s1, None, op0), r, w)
        return self.op(eng, lambda e: e.tensor_scalar(out, in0, s1, s2, op0, op1), r, w)

    def tt(self, eng, out, in0, in1, op, r, w):
        return self.op(eng, lambda e: e.tensor_tensor(out, in0, in1, op), r, w)

    def stt(self, out, in0, scalar, in1, op0, op1, r, w):
        return self.op("dve", lambda e: e.scalar_tensor_tensor(out, in0, scalar, in1, op0, op1), r, w)

    def copy(self, eng, out, in_, r, w):
        if eng == "act":
            return self.op("act", lambda e: e.copy(out, in_), r, w)
        return self.op(eng, lambda e: e.tensor_copy(out, in_), r, w)

    def ld(self, out, in_, semkey, r, w, queue="sp"):
        return self.dma(queue, lambda e: e.dma_start(out, in_), semkey, r, w)

    def st(self, out, in_, semkey, r, w, queue="pool", is_out=False):
        return self.dma(queue, lambda e: e.dma_start(out, in_), semkey, r, w, is_out=is_out)


_UNIQ = [0]
DECL_INPUTS = []


def uniq(name):
    _UNIQ[0] += 1
    return f"sb{_UNIQ[0]}_{name}"


def bcast(ap, shape):
    return ap.to_broadcast(shape)


def build(stage="full", dbg=False):
    nc = bass.Bass("TRN2", target_bir_lowering=False)
    es = ExitStack()
    with es:
        _build(nc, es, stage, dbg)
    return nc


def _build(nc, es, stage, dbg):
    P = Prog(nc, es)

    DECL_INPUTS.clear()

    def dram_in(name, shape, dt=F32):
        DECL_INPUTS.append(name)
        return nc.dram_tensor(name, list(shape), dt, kind="ExternalInput").ap()

    def dram_scr(name, shape, dt=BF16):
        kind = "ExternalOutput" if (dbg and name in DBG_OUT) else "Internal"
        return nc.dram_tensor(name, list(shape), dt, kind=kind).ap()

    def sb(name, shape, dt=F32):
        return es.enter_context(nc.sbuf_tensor(uniq(name), list(shape), dt))

    x_ext = dram_in("x_ext", [TEXT, D])
    w_in = dram_in("w_in", [D, DIN])
    g_mix_t = dram_in("g_mix_t", [128, 8])
    ident_d = dram_in("ident", [128, 128])
    out_d = nc.dram_tensor("out", [TOWN, D], F32, kind="ExternalOutput").ap()

    QGT = dram_scr("QGT", [64, 4, TOWN])
    KGT = dram_scr("KGT", [64, 4, TEXT])
    AT = dram_scr("AT", [16, TEXT])
    QMT = dram_scr("QMT", [8, 64, TOWN])
    KMT = dram_scr("KMT", [8, 64, TEXT])
    ZGT = dram_scr("ZGT", [8, 128, TOWN])
    ZMT = dram_scr("ZMT", [8, 128, TOWN])
    VG = dram_scr("VG", [TEXT, 512])
    KG = dram_scr("KG", [TEXT, 256])
    RG = dram_scr("RG", [TOWN, 512])
    VM = dram_scr("VM", [TEXT, 512])

    ps_all = es.enter_context(nc.psum_tensor("ps_all", [128, 4096], F32))
    ps = [ps_all[:, i * 512:(i + 1) * 512] for i in range(8)]
    PSK = [f"ps{i}" for i in range(8)]

    ident_f = sb("ident_f", [128, 128])
    ident_b = sb("ident_b", [128, 128], BF16)
    P.ld(ident_f[:], ident_d, "c0", [], ["ident_f"])
    P.copy("dve", ident_b[:], ident_f[:], ["ident_f"], ["ident_b"])
    gmix = sb("gmix", [128, 8])
    P.ld(gmix[:], g_mix_t, "c1", [], ["gmix"])

    with ExitStack() as pes:
        def psb(name, shape, dt=F32):
            return pes.enter_context(nc.sbuf_tensor(uniq(name), list(shape), dt))
        win_b = psb("win_b", [128, 8, DIN], BF16)
        w_in_v = w_in.rearrange("(k p) n -> p k n", p=128)
        wblocks = [(C_KG, 256), (C_A, 16), (C_KM, 512), (C_VG, 512), (C_VM, 512),
                   (C_QG, 256), (C_QM, 512), (C_ZG, 512), (C_ZG + 512, 512), (C_ZM, 512), (C_ZM + 512, 512), (C_RG, 512)]
        wkeys = {}
        for bi, (c0_, n_) in enumerate(wblocks):
            for c_ in range(c0_, c0_ + n_, 16):
                wkeys[c_] = f"win{bi}"
            P.dma("pool", lambda e, c0_=c0_, n_=n_: e.dma_start(win_b[:, :, c0_:c0_ + n_], w_in_v[:, :, c0_:c0_ + n_]),
                  f"winq{bi}", [], [f"win{bi}"])

        def wkey(c0_, n_):
            return sorted({wkeys[c_] for c_ in range(c0_, c0_ + n_, 16)})

        xt = [psb(f"xt{i}", [128, D]) for i in range(4)]
        junk = psb("junk", [128, D], BF16)
        xn = [psb(f"xn{i}", [128, D], BF16) for i in range(4)]
        stat = [psb(f"stat{i}", [128, 4]) for i in range(4)]
        hT = [psb(f"hT{i}", [128, 8, 512], BF16) for i in range(2)]
        s_qg = psb("s_qg", [128, 2, 512], BF16)
        s_kg = psb("s_kg", [128, 2, 512], BF16)
        s_a = psb("s_a", [16, 512], BF16)
        s_qm = psb("s_qm", [128, 4, 512], BF16)
        s_km = psb("s_km", [128, 4, 512], BF16)
        s_zg = psb("s_zg", [128, 8, 512], BF16)
        s_zm = psb("s_zm", [128, 8, 512], BF16)
        s_vg = psb("s_vg", [128, 4, 512], BF16)
        s_kgt = psb("s_kgt", [128, 4, 256], BF16)
        s_rg = psb("s_rg", [128, 4, 512], BF16)
        s_vm = psb("s_vm", [128, 4, 512], BF16)

        x_v = x_ext.rearrange("(n p) d -> n p d", p=128)
        ti = 0
        pbank = [0]

        def nextbank():
            b = pbank[0]
            pbank[0] = (b + 1) % 7
            return b

        evq = [0]

        def evac(out, in_, r, w, scale=None):
            e = ["act", "dve"][evq[0] % 2]
            evq[0] += 1
            if scale is None:
                P.copy(e, out, in_, r, w)
            elif e == "act":
                P.act("act", out, in_, AF.Copy, r, w, scale=scale)
            else:
                P.ts("dve", out, in_, scale, None, ALU.mult, None, r, w)

        NG = TEXT // 512

        def ab_stats(g):
            for t in range(4):
                i = g * 4 + t
                P.ld(xt[t][:], x_v[i], f"xt{t}", [], [f"xt{t}"])
                P.act("act", junk[:], xt[t][:], AF.Square, [f"xt{t}"], ["junk", f"stat{t}"], accum=stat[t][:, 0:1])
                P.ts("dve", stat[t][:, 1:2], stat[t][:, 0:1], 1.0 / D, EPS, ALU.mult, ALU.add, [f"stat{t}"], [f"stat{t}b"])
                P.act("act", stat[t][:, 2:3], stat[t][:, 1:2], AF.Ln, [f"stat{t}b"], [f"stat{t}c"])
                P.act("act", stat[t][:, 3:4], stat[t][:, 2:3], AF.Exp, [f"stat{t}c"], [f"stat{t}d"], scale=-0.5)
                P.ts("dve", xn[t][:], xt[t][:], stat[t][:, 3:4], None, ALU.mult, None, [f"xt{t}", f"stat{t}d"], [f"xn{t}"])

        def ab_tr(g):
            hs = g % 2
            hk = f"hT{hs}"
            for t in range(4):
                pt = ps[7][:].bitcast(BF16)
                for k in range(8):
                    P.tr(pt[:, k * 128:(k + 1) * 128], xn[t][:, k * 128:(k + 1) * 128], ident_b[:],
                         [f"xn{t}", "ident_b"], ["ps7"])
                P.tt("dve", hT[hs][:, :, t * 128:(t + 1) * 128], pt.rearrange("p (k t) -> p k t", k=8),
                     bcast(gmix[:].unsqueeze(2), [128, 8, 128]), ALU.mult, ["ps7", "gmix"], [hk])

        def ab_parts(g):
            own = g >= NG // 2
            go = g - NG // 2
            hs = g % 2
            hk = f"hT{hs}"
            parts = []

            def fm(c0, m, dst, dkey, scale=None):
                def f_():
                    b = nextbank()
                    for k in range(8):
                        P.mm(ps[b][0:m, :], win_b[:, k, c0:c0 + m], hT[hs][:, k, :], k == 0, k == 7,
                             wkey(c0, m) + [hk], [PSK[b]])
                    evac(dst, ps[b][0:m, :], [PSK[b]], [dkey], scale)
                parts.append(f_)

            def tm(c0, n, dst3, dkey):
                for t in range(4):
                    def f_(t=t):
                        b = nextbank()
                        for k in range(8):
                            P.mm(ps[b][:, 0:n], hT[hs][:, k, t * 128:(t + 1) * 128], win_b[:, k, c0:c0 + n], k == 0, k == 7,
                                 wkey(c0, n) + [hk], [PSK[b]])
                        evac(dst3[:, t, :], ps[b][:, 0:n], [PSK[b]], [dkey])
                    parts.append(f_)

            def st_(fn):
                parts.append(fn)

            tsl = slice(g * 512, (g + 1) * 512)
            osl = slice(go * 512, (go + 1) * 512)
            for pr in range(2):
                fm(C_KG + pr * 128, 128, s_kg[:, pr, :], "s_kg")
            st_(lambda: [P.st(KGT.rearrange("d (pr hh) t -> d hh pr t", hh=2)[:, hh, :, tsl], s_kg[hh * 64:(hh + 1) * 64, :, :], "s_kg", ["s_kg"], ["KGT"]) for hh in range(2)])
            fm(C_A, 16, s_a[:], "s_a")
            st_(lambda: P.st(AT[:, tsl], s_a[:], "s_a", ["s_a"], ["AT"]))
            for pr in range(4):
                fm(C_KM + pr * 128, 128, s_km[:, pr, :], "s_km")
            st_(lambda: [P.st(KMT.rearrange("(pr hh) d t -> hh d pr t", hh=2)[hh][:, :, tsl], s_km[hh * 64:(hh + 1) * 64, :, :], "s_km", ["s_km"], ["KMT"]) for hh in range(2)])
            tm(C_VG, 512, s_vg, "s_vg")
            st_(lambda: P.st(VG[tsl, :].rearrange("(t p) f -> p t f", p=128), s_vg[:], "s_vg", ["s_vg"], ["VG"]))
            tm(C_KG, 256, s_kgt, "s_kgt")
            st_(lambda: P.st(KG[tsl, :].rearrange("(t p) f -> p t f", p=128), s_kgt[:], "s_kgt", ["s_kgt"], ["KG"]))
            tm(C_VM, 512, s_vm, "s_vm")
            st_(lambda: P.st(VM[tsl, :].rearrange("(t p) f -> p t f", p=128), s_vm[:], "s_vm", ["s_vm"], ["VM"]))
            if own:
                for pr in range(2):
                    fm(C_QG + pr * 128, 128, s_qg[:, pr, :], "s_qg")
                st_(lambda: [P.st(QGT.rearrange("d (pr hh) t -> d hh pr t", hh=2)[:, hh, :, osl], s_qg[hh * 64:(hh + 1) * 64, :, :], "s_qg", ["s_qg"], ["QGT"]) for hh in range(2)])
                for pr in range(4):
                    fm(C_QM + pr * 128, 128, s_qm[:, pr, :], "s_qm", scale=0.125)
                st_(lambda: [P.st(QMT.rearrange("(pr hh) d t -> hh d pr t", hh=2)[hh][:, :, osl], s_qm[hh * 64:(hh + 1) * 64, :, :], "s_qm", ["s_qm"], ["QMT"]) for hh in range(2)])
                for c in range(8):
                    fm(C_ZG + c * 128, 128, s_zg[:, c, :], "s_zg")
                st_(lambda: P.st(ZGT[:, :, osl].rearrange("c p t -> p c t"), s_zg[:], "s_zg", ["s_zg"], ["ZGT"]))
                for c in range(8):
                    fm(C_ZM + c * 128, 128, s_zm[:, c, :], "s_zm")
                st_(lambda: P.st(ZMT[:, :, osl].rearrange("c p t -> p c t"), s_zm[:], "s_zm", ["s_zm"], ["ZMT"]))
                tm(C_RG, 512, s_rg, "s_rg")
                st_(lambda: P.st(RG[osl, :].rearrange("(t p) f -> p t f", p=128), s_rg[:], "s_rg", ["s_rg"], ["RG"]))
            return parts

        ab_stats(0)
        ab_tr(0)
        for g in range(NG):
            parts = ab_parts(g)
            if g + 1 < NG:
                ab_stats(g + 1)
            half = len(parts) // 2
            for f_ in parts[:half]:
                f_()
            if g + 1 < NG:
                ab_tr(g + 1)
            for f_ in parts[half:]:
                f_()
        P.barrier()

    def stop_here():
        z = sb("zout_" + stage, [128, D])
        P.op("dve", lambda e: e.memset(z[:], 0.0), [], ["zout"])
        P.st(out_d[0:128, :], z[:], "zo", ["zout"], ["out"], is_out=True)
        P.finish()

    if stage == "AB":
        stop_here()
        return

    def load_cast(dst, src, n, stg_tiles, ci, dkey, parts=128, engs=("pool", "dve", "act")):
        s = ci[0] % len(stg_tiles)
        shp = list(dst.shape)
        F = 1
        for d_ in shp[1:]:
            F *= d_
        sv = stg_tiles[s][0:parts, 0:F]
        if len(shp) == 3:
            sv = sv.rearrange("p (a b) -> p a b", a=shp[1])
        P.ld(sv, src, f"wstg{s}", [], [f"wstg{s}"])
        P.copy(engs[ci[0] % len(engs)], dst, sv, [f"wstg{s}"], [dkey])
        ci[0] += 1

    XS = dram_scr("XS", [NROWS, D], BF16)
    zt = sb("zt", [128, D], BF16)
    P.op("pool", lambda e: e.memset(zt[:], 0.0), [], ["zt"])
    XSv = XS.rearrange("(t p) d -> t p d", p=128)
    zfill_i = [0]

    def zfill(n):
        for _ in range(n):
            if zfill_i[0] < NROWS // 128:
                P.st(XSv[zfill_i[0]], zt[:], "zfill", ["zt"], ["XS"], queue="sp")
                zfill_i[0] += 1

    w_up_d = dram_in("w_alpha_up", [16, 256])
    b_al_d = dram_in("b_alpha", [1, 256])
    u2_d = dram_in("u2", [128, 128])
    u3_d = dram_in("u3", [128, 128])
    maskT_d = dram_in("maskT", [64, 64])
    ggla_d = dram_in("ggla_rep", [64, 512])
    OGT = dram_scr("OGT", [4, 128, TOWN])
    with ExitStack() as pes:
        def psb(name, shape, dt=F32):
            return pes.enter_context(nc.sbuf_tensor(uniq(name), list(shape), dt))
        wup_f = psb("wup_f", [16, 256]); wup_b = psb("wup_b", [16, 256], BF16)
        bal_f = psb("bal_f", [1, 256]); bal_b = psb("bal_b", [1, 256], BF16)
        ones_row = psb("ones_row", [1, 128], BF16)
        U2 = psb("U2", [128, 128]); maskT = psb("maskT", [64, 64]); ggla = psb("ggla", [64, 512])
        U3 = psb("U3", [128, 128])
        P.ld(U3[:], u3_d, "k5", [], ["U3"])
        P.ld(wup_f[:], w_up_d, "k0", [], ["wup_f"]); P.copy("dve", wup_b[:], wup_f[:], ["wup_f"], ["wup_b"])
        P.ld(bal_f[:], b_al_d, "k1", [], ["bal_f"]); P.copy("dve", bal_b[:], bal_f[:], ["bal_f"], ["bal_b"])
        P.op("dve", lambda e: e.memset(ones_row[:], 1.0), [], ["ones_row"])
        P.ld(U2[:], u2_d, "k2", [], ["U2"]); P.ld(maskT[:], maskT_d, "k3", [], ["maskT"]); P.ld(ggla[:], ggla_d, "k4", [], ["ggla"])
        state_f = psb("state_f", [64, 512]); state_b = psb("state_b", [64, 4, 128], BF16)
        P.op("dve", lambda e: e.memset(state_f[:], 0.0), [], ["state_f"])
        P.op("dve", lambda e: e.memset(state_b[:], 0.0), [], ["state_b"])
        kTg = [psb(f"kTg{i}", [64, 4, 512], BF16) for i in range(2)]
        qTg = [psb(f"qTg{i}", [64, 4, 512], BF16) for i in range(2)]
        aTg = [psb(f"aTg{i}", [16, 512], BF16) for i in range(2)]
        vg = [psb(f"vg{i}", [64, 8, 512], BF16) for i in range(2)]
        kg = [psb(f"kg{i}", [64, 8, 256], BF16) for i in range(2)]
        rg = [psb(f"rg{i}", [64, 8, 512], BF16) for i in range(2)]
        ogTg = [psb(f"ogTg{i}", [128, 4, 512], BF16) for i in range(2)]
        e1 = psb("e1", [128, 256]); nla = psb("nla", [128, 256])
        EBt = [psb(f"EBt{i}", [64, 512]) for i in range(3)]
        EpT = [psb(f"EpT{i}", [64, 512]) for i in range(3)]
        EmT = [psb(f"EmT{i}", [64, 512]) for i in range(3)]
        Kpt = [psb(f"Kpt{i}", [64, 2, 256], BF16) for i in range(3)]
        KpT = [psb(f"KpT{i}", [64, 4, 128], BF16) for i in range(3)]
        QpT = [psb(f"QpT{i}", [64, 4, 128], BF16) for i in range(3)]
        attT = psb("attT", [64, 4, 64], BF16)
        o_sb2 = [psb(f"o_sb{i}", [64, 2, 512]) for i in range(2)]; tmp = psb("tmp", [64, 512])
        sq = psb("sq", [64, 1024]); st8 = psb("st8", [64, 32])
        og1 = psb("og1", [64, 1024]); og2 = psb("og2", [64, 1024]); sr = psb("sr", [64, 1024]); og = psb("og", [64, 2, 512], BF16)
        NG = TEXT // 512
        tiles = [(g, t) for g in range(NG) for t in range(4)]

        def c_loads(g):
            own = g >= NG // 2
            go = g - NG // 2
            s = g % 2
            tsl = slice(g * 512, (g + 1) * 512)
            osl = slice(go * 512, (go + 1) * 512)
            P.ld(kTg[s][:], KGT[:, :, tsl], f"kTg{s}", ["KGT"], [f"kTg{s}"])
            P.ld(aTg[s][:], AT[:, tsl], f"aTg{s}", ["AT"], [f"aTg{s}"])
            P.ld(vg[s][:], VG[tsl, :].rearrange("(c p) f -> p c f", p=64), f"vg{s}", ["VG"], [f"vg{s}"])
            P.ld(kg[s][:], KG[tsl, :].rearrange("(c p) f -> p c f", p=64), f"kg{s}", ["KG"], [f"kg{s}"])
            if own:
                P.ld(qTg[s][:], QGT[:, :, osl], f"qTg{s}", ["QGT"], [f"qTg{s}"])
                P.ld(rg[s][:], RG[osl, :].rearrange("(c p) f -> p c f", p=64), f"rg{s}", ["RG"], [f"rg{s}"])

        def ph1(idx):
            g, t = tiles[idx]
            own = g >= NG // 2
            s = g % 2
            p = idx % 3
            tc = slice(t * 128, (t + 1) * 128)
            P.mm(ps[0][:, 0:256], aTg[s][:, tc], wup_b[:], True, False, [f"aTg{s}", "wup_b"], ["ps0"])
            P.mm(ps[0][:, 0:256], ones_row[:], bal_b[:], False, True, ["ones_row", "bal_b"], ["ps0"])
            P.act("act", e1[:], ps[0][:, 0:256], AF.Exp, ["ps0"], ["e1"], scale=-1.0)
            P.act("act", nla[:], e1[:], AF.Ln, ["e1"], ["nla"], bias=1.0)
            for j in range(2):
                P.mm(ps[1][0:64, j * 256:(j + 1) * 256], U3[:, j * 64:(j + 1) * 64], nla[:], True, True, ["U3", "nla"], ["ps1"])
            for h in range(4):
                P.mm(ps[2][0:64, h * 128:(h + 1) * 128], nla[:, h * 64:(h + 1) * 64], U2[:], True, True, ["U2", "nla"], ["ps2"])
            P.act("act", EBt[p][:], ps[1][0:64, :], AF.Exp, ["ps1"], [f"EBt{p}"], scale=-1.0 / 16)
            P.tt("dve", Kpt[p][:], kg[s][:, 2 * t:2 * t + 2, :], EBt[p][:].rearrange("p (j f) -> p j f", j=2), ALU.mult,
                 [f"kg{s}", f"EBt{p}"], [f"Kpt{p}"])
            P.act("act", EmT[p][:], ps[2][0:64, :], AF.Exp, ["ps2"], [f"EmT{p}"], scale=-1.0 / 16)
            if own:
                P.act("act", EpT[p][:], ps[2][0:64, :], AF.Exp, ["ps2"], [f"EpT{p}"], scale=1.0 / 16)
                P.tt("pool", KpT[p][:], kTg[s][:, :, tc], EpT[p][:].rearrange("p (h c) -> p h c", h=4), ALU.mult,
                     [f"kTg{s}", f"EpT{p}"], [f"KpT{p}"])
                P.stt(QpT[p][:], qTg[s][:, :, tc], 0.125, EmT[p][:].rearrange("p (h c) -> p h c", h=4), ALU.mult, ALU.mult,
                      [f"qTg{s}", f"EmT{p}"], [f"QpT{p}"])

        def ph2(idx):
            g, t = tiles[idx]
            own = g >= NG // 2
            go = g - NG // 2
            s = g % 2
            p = idx % 3
            tc = slice(t * 128, (t + 1) * 128)
            EmT3 = EmT[p][:].rearrange("p (h c) -> p h c", h=4)
            o_sb = o_sb2[idx % 2]
            ok_ = f"o_sb{idx % 2}"
            for j in range(2):
                cc = slice(j * 64, (j + 1) * 64)
                ch = 2 * t + j
                if own:
                    for h in range(4):
                        P.mm(ps[3][0:64, h * 64:(h + 1) * 64], KpT[p][:, h, cc], QpT[p][:, h, cc], True, True, [f"KpT{p}", f"QpT{p}"], ["ps3"])
                    P.tt("dve", attT[:], ps[3][0:64, 0:256].rearrange("p (h c) -> p h c", h=4),
                         bcast(maskT[:].unsqueeze(1), [64, 4, 64]), ALU.mult, ["ps3", "maskT"], ["attT"])
                    for h in range(4):
                        P.mm(ps[4][0:64, h * 128:(h + 1) * 128], attT[:, h, :], vg[s][:, ch, h * 128:(h + 1) * 128], True, False,
                             ["attT", f"vg{s}"], ["ps4"])
                        P.mm(ps[4][0:64, h * 128:(h + 1) * 128], QpT[p][:, h, cc], state_b[:, h, :], False, True,
                             [f"QpT{p}", "state_b"], ["ps4"])
                    P.copy("act", o_sb[:, j, :], ps[4][0:64, :], ["ps4"], [ok_])
                for h in range(4):
                    P.mm(ps[5][0:64, h * 128:(h + 1) * 128], Kpt[p][:, j, h * 64:(h + 1) * 64], vg[s][:, ch, h * 128:(h + 1) * 128], True, True,
                         [f"Kpt{p}", f"vg{s}"], ["ps5"])
                dec = bcast(EmT3[:, :, j * 64 + 63:j * 64 + 64], [64, 4, 128])
                tmp3 = tmp[:].rearrange("p (h c) -> p h c", h=4)
                P.tt("dve", tmp3, state_f[:].rearrange("p (h c) -> p h c", h=4), dec, ALU.mult, ["state_f", f"EmT{p}"], ["tmp"])
                P.tt("dve", state_b[:].rearrange("p h c -> p (h c)"), tmp[:], ps[5][0:64, :], ALU.add, ["tmp", "ps5"], ["state_b"])
                P.tt("dve", state_f[:], tmp[:], ps[5][0:64, :], ALU.add, ["tmp", "ps5"], ["state_f"])

        def ph3(idx):
            g, t = tiles[idx]
            own = g >= NG // 2
            go = g - NG // 2
            s = g % 2
            tc = slice(t * 128, (t + 1) * 128)
            o_sb = o_sb2[idx % 2]
            ok_ = f"o_sb{idx % 2}"
            if own:
                of = o_sb[:].rearrange("p j f -> p (j f)")
                P.tt("pool", sq[:], of, of, ALU.mult, [ok_], ["sq"])
                P.op("dve", lambda e: e.tensor_reduce(st8[:, 0:8], sq[:].rearrange("p (a b) -> p a b", a=8), AX.X, ALU.add),
                     ["sq"], ["st8a"])
                P.ts("dve", st8[:, 8:16], st8[:, 0:8], 1.0 / 128, EPS, ALU.mult, ALU.add, ["st8a"], ["st8b"])
                P.act("act", st8[:, 16:24], st8[:, 8:16], AF.Ln, ["st8b"], ["st8c"])
                P.act("act", st8[:, 24:32], st8[:, 16:24], AF.Exp, ["st8c"], ["st8d"], scale=-0.5)
                P.tt("dve", og1[:].rearrange("p (a b) -> p a b", a=8), of.rearrange("p (a b) -> p a b", a=8),
                     bcast(st8[:, 24:32].unsqueeze(2), [64, 8, 128]), ALU.mult, [ok_, "st8d"], ["og1"])
                P.tt("pool", og2[:].rearrange("p (j f) -> p j f", j=2), og1[:].rearrange("p (j f) -> p j f", j=2),
                     bcast(ggla[:].unsqueeze(1), [64, 2, 512]), ALU.mult, ["og1", "ggla"], ["og2"])
                P.act("act", sr[:].rearrange("p (j f) -> p j f", j=2), rg[s][:, 2 * t:2 * t + 2, :], AF.Silu, [f"rg{s}"], ["sr"])
                P.tt("dve", og[:].rearrange("p j f -> p (j f)"), og2[:], sr[:], ALU.mult, ["og2", "sr"], ["og"])
                pt = ps[6][:].bitcast(BF16)
                for kc in range(4):
                    for j in range(2):
                        P.tr(pt[:, kc * 128 + j * 64:kc * 128 + (j + 1) * 64], og[:, j, kc * 128:(kc + 1) * 128], ident_b[0:64, 0:64],
                             ["og", "ident_b"], ["ps6"])
                P.copy("act", ogTg[s][:, :, tc], pt[:, 0:512].rearrange("p (k c) -> p k c", k=4), ["ps6"], [f"ogTg{s}"])
                if t == 3:
                    osl = slice(go * 512, (go + 1) * 512)
                    P.st(OGT[:, :, osl].rearrange("c p t -> p c t"), ogTg[s][:], f"ogTg{s}", [f"ogTg{s}"], ["OGT"])

        c_loads(0)
        zfill(6)
        ph1(0)
        ph1(1)
        for idx in range(len(tiles)):
            if idx + 2 < len(tiles):
                g2, t2 = tiles[idx + 2]
                if t2 == 0:
                    c_loads(g2)
                    zfill(6)
                ph1(idx + 2)
            ph2(idx)
            if idx >= 1:
                ph3(idx - 1)
        ph3(len(tiles) - 1)
        P.barrier()
    if stage == "C":
        stop_here()
        return

    wpg_d = dram_in("w_proj_gla", [512, D]); wpm_d = dram_in("w_proj_moba", [512, D]); wout_d = dram_in("w_out", [D, D])
    wpg_b = sb("wpg_b", [128, 4, D], BF16); wpm_b = sb("wpm_b", [128, 4, D], BF16); wout_b = sb("wout_b", [128, 8, D], BF16)
    P.dma("pool", lambda e: e.dma_start(wpg_b[:], wpg_d.rearrange("(k p) n -> p k n", p=128)), "pfE0", [], ["wpg_b"])
    P.dma("pool", lambda e: e.dma_start(wpm_b[:], wpm_d.rearrange("(k p) n -> p k n", p=128)), "pfE1", [], ["wpm_b"])
    for hh_ in range(2):
        P.dma("pool", lambda e, hh_=hh_: e.dma_start(wout_b[:, hh_ * 4:(hh_ + 1) * 4, :],
              wout_d[hh_ * 512:(hh_ + 1) * 512, :].rearrange("(k p) n -> p k n", p=128)), "pfE2", [], ["wout_b"])
    ind_d = dram_in("ind", [32, TEXT], BF16)
    tb_d = dram_in("tb", [8, 128, 6, 512], BF16)
    gadd_d = dram_in("gadd", [128, 32, 32])
    pv01_d = dram_in("pv01", [128, 32, 32])
    bfix_d = dram_in("bfix", [128, 32, 32])
    cfm_d = dram_in("cfm", [128, 32, 32])
    cfar_d = dram_in("cfar_rep", [128, 8])
    sh_d = dram_in("sh", [128, 64])
    OMT = dram_scr("OMT", [4, 128, TOWN])
    with ExitStack() as pes:
        def psb(name, shape, dt=F32):
            return pes.enter_context(nc.sbuf_tensor(uniq(name), list(shape), dt))
        kTa = [psb(f"kTa{i}", [96, TEXT], BF16) for i in range(2)]
        Va = [psb(f"Va{i}", [128, 64, 128], BF16) for i in range(2)]
        qTa = [psb(f"qTa{i}", [96, TOWN], BF16) for i in range(2)]
        Tb = [psb(f"Tb{i}", [128, 6, 512], BF16) for i in range(2)]
        gadd = psb("gadd", [128, 32, 32]); pv01 = psb("pv01", [128, 32, 32]); bfix = psb("bfix", [128, 32, 32]); cfm = psb("cfm", [128, 32, 32])
        cfar = psb("cfar", [128, 8]); Sh = psb("Sh", [128, 64])
        P.ld(gadd[:], gadd_d, "m0", [], ["gadd"]); P.ld(pv01[:], pv01_d, "m1", [], ["pv01"])
        P.ld(bfix[:], bfix_d, "m2", [], ["bfix"]); P.ld(cfm[:], cfm_d, "m3", [], ["cfm"])
        P.ld(cfar[:], cfar_d, "m4", [], ["cfar"]); P.ld(Sh[:], sh_d, "m5", [], ["Sh"])
        for i in range(2):
            P.ld(kTa[i][64:96, :], ind_d, f"ind{i}", [], [f"kTa{i}i"])
            P.op("pool", lambda e, i=i: e.memset(Va[i][:, :, 64:128], 1.0), [], [f"Va{i}o"])
        kmf = psb("kmf", [64, 32]); kmb = psb("kmb", [64, 32], BF16)
        gs = psb("gs", [128, 512]); g2 = psb("g2", [128, 512]); eqt = psb("eqt", [128, 512]); mx = psb("mx", [128, 48]); acoef = psb("acoef", [128, 1024])
        MBw2 = [psb(f"MBw{i}", [128, 16, 96], BF16) for i in range(2)]
        for i in range(2):
            P.op("dve", lambda e, i=i: e.memset(MBw2[i][:], 0.0), [], [f"MBw{i}"])
        PT = [psb(f"PT{i}", [128, 1024], BF16) for i in range(3)]
        obs = [psb(f"obs{i}", [128, 512]) for i in range(2)]
        rec2 = [psb(f"rec{i}", [128, 512]) for i in range(2)]
        pending_tail = []
        omT = [psb(f"omT{i}", [64, 512], BF16) for i in range(2)]
        for i in range(2):
            P.op("dve", lambda e, i=i: e.memset(rec2[i][:], 0.0), [], [f"rec{i}"])
        pti = 0
        sbk = 0

        def d_loads(h):
            s = h % 2
            P.ld(kTa[s][0:64, :], KMT[h], f"kTa{s}", ["KMT"], [f"kTa{s}"])
            for qq in range(4):
                P.ld(Va[s][:, qq * 16:(qq + 1) * 16, 0:64],
                     VM[qq * 2048:(qq + 1) * 2048, h * 64:(h + 1) * 64].rearrange("(t p) f -> p t f", p=128),
                     f"Va{s}", ["VM"], [f"Va{s}"])
            P.ld(qTa[s][0:64, :], QMT[h], f"qTa{s}", ["QMT"], [f"qTa{s}"])
            P.ld(Tb[s][:], tb_d[h], f"Tb{s}", [], [f"Tb{s}"])

        def d_kmean(h):
            s = h % 2
            P.op("dve", lambda e, s=s: e.tensor_reduce(kmf[:], kTa[s][0:64, :].rearrange("p (n k) -> p n k", n=32), AX.X, ALU.add),
                 [f"kTa{s}"], ["kmf"])
            P.copy("dve", kmb[:], kmf[:], ["kmf"], ["kmb"])
            P.ts("pool", acoef[:], cfm[:].rearrange("p a b -> p (a b)"), cfar[:, h:h + 1], -NEG, ALU.mult, ALU.add, ["cfm", "cfar"], ["acoef"])

        def gate_p1(h, hb):
            s = h % 2
            q0 = hb * 16
            for qi in range(16):
                qc = slice((q0 + qi) * 128, (q0 + qi + 1) * 128)
                P.mm(ps[7][:, qi * 32:(qi + 1) * 32], qTa[s][0:64, qc], kmb[:], True, True, [f"qTa{s}", "kmb"], ["ps7"])
            gs3 = gs[:].rearrange("p (a b) -> p a b", a=16)
            g23 = g2[:].rearrange("p (a b) -> p a b", a=16)
            eq3 = eqt[:].rearrange("p (a b) -> p a b", a=16)
            P.tt("dve", gs3, ps[7][:, :].rearrange("p (a b) -> p a b", a=16), gadd[:, q0:q0 + 16, :], ALU.add, ["ps7", "gadd"], ["gs"])
            P.op("dve", lambda e, gs3=gs3: e.tensor_reduce(mx[:, 0:16], gs3, AX.X, ALU.max), ["gs"], ["mx0"])
            P.tt("dve", eq3, gs3, bcast(mx[:, 0:16].unsqueeze(2), [128, 16, 32]), ALU.is_ge, ["gs", "mx0"], ["eqt"])
            P.stt(g2[:], eqt[:], -1e30, gs[:], ALU.mult, ALU.add, ["eqt", "gs"], ["g2"])
            P.op("dve", lambda e, g23=g23: e.tensor_reduce(mx[:, 16:32], g23, AX.X, ALU.max), ["g2"], ["mx1"])
            P.tt("dve", eq3, g23, bcast(mx[:, 16:32].unsqueeze(2), [128, 16, 32]), ALU.is_ge, ["g2", "mx1"], ["eqt"])
            P.stt(g2[:], eqt[:], -1e30, g2[:], ALU.mult, ALU.add, ["eqt", "g2"], ["g2"])
            P.op("dve", lambda e, g23=g23: e.tensor_reduce(mx[:, 32:48], g23, AX.X, ALU.max), ["g2"], ["mx2"])
            P.tt("dve", eq3, gs3, bcast(mx[:, 32:48].unsqueeze(2), [128, 16, 32]), ALU.is_ge, ["gs", "mx2"], ["eqt"])
            P.tt("dve", eq3, eq3, pv01[:, q0:q0 + 16, :], ALU.mult, ["eqt", "pv01"], ["eqt"])
            P.tt("dve", eq3, eq3, acoef[:].rearrange("p (a b) -> p a b", a=32)[:, q0:q0 + 16, :], ALU.mult, ["eqt", "acoef"], ["eqt"])
            P.tt("dve", MBw2[hb][:, :, 64:96], eq3, bfix[:, q0:q0 + 16, :], ALU.add, ["eqt", "bfix"], [f"MBw{hb}"])

        def gate_p2(h, hb):
            s = h % 2
            q0 = hb * 16
            for half in range(2):
                pt = ps[7][:].bitcast(BF16)
                for qi in range(8):
                    P.tr(pt[0:96, qi * 128:(qi + 1) * 128], MBw2[hb][:, half * 8 + qi, :], ident_b[:], [f"MBw{hb}", "ident_b"], ["ps7"])
                c0 = (q0 + half * 8) * 128
                P.copy("act", qTa[s][64:96, c0:c0 + 1024], pt[64:96, :], ["ps7"], [f"qTa{s}m"])

        d_loads(0)
        d_kmean(0)
        gate_p1(0, 0); gate_p2(0, 0); gate_p1(0, 1); gate_p2(0, 1)
        for h in range(8):
            s = h % 2
            if h + 1 < 8:
                d_loads(h + 1)
            for g in range(8):
                osl = slice(g * 512, (g + 1) * 512)
                nkt = 32 + 4 * g + 4
                OB = 6
                npair = nkt // 2

                def emitS(pi):
                    nonlocal sbk
                    db = sbk % 3
                    sbk += 1
                    sdb[pi] = db
                    for u in range(2):
                        kt = 2 * pi + u
                        b = 2 * db + u
                        j = kt - (nkt - 6)
                        P.mm(ps[b][:, :], kTa[s][:, kt * 128:(kt + 1) * 128], qTa[s][:, osl], True, j < 0,
                             [f"kTa{s}", f"kTa{s}i", f"qTa{s}", f"qTa{s}m"], [f"psd{db}"])
                        if j >= 0:
                            P.mm(ps[b][:, :], ident_b[:], Tb[s][:, j, :], False, True, ["ident_b", f"Tb{s}"], [f"psd{db}"])

                sdb = {}
                emitS(0)
                emitS(1)
                for pi in range(npair):
                    if pi + 2 < npair:
                        emitS(pi + 2)
                    if pi == 2:
                        while pending_tail:
                            pending_tail.pop(0)()
                    db = sdb[pi]
                    p = pti % 3
                    pti += 1
                    P.act("act", PT[p][:], ps_all[:, db * 1024:(db + 1) * 1024], AF.Exp, [f"psd{db}"], [f"PT{p}"])
                    for u in range(2):
                        kt = 2 * pi + u
                        P.mm(ps[OB][:, :], Va[s][:, kt, :], PT[p][:, u * 512:(u + 1) * 512], kt == 0, kt == nkt - 1,
                             [f"Va{s}", f"Va{s}o", f"PT{p}"], [PSK[OB]])
                o2 = g % 2
                P.copy("dve", obs[o2][:], ps[OB][:, :], [PSK[OB]], [f"obs{o2}"])
                P.op("dve", lambda e, o2=o2: e.reciprocal(rec2[o2][64:128, :], obs[o2][64:128, :]), [f"obs{o2}"], [f"rec{o2}"])

                def tail(h=h, o2=o2, osl=osl):
                    P.mm(ps[7][0:64, :], Sh[:], rec2[o2][:], True, True, ["Sh", f"rec{o2}"], ["ps7"])
                    P.tt("dve", omT[o2][:], ps[7][0:64, :], obs[o2][0:64, :], ALU.mult, ["ps7", f"obs{o2}"], [f"omT{o2}"])
                    P.st(OMT[h // 2, (h % 2) * 64:(h % 2) * 64 + 64, osl], omT[o2][:], f"omT{o2}", [f"omT{o2}"], ["OMT"])
                pending_tail.append(tail)
                if h + 1 < 8:
                    if g == 0:
                        d_kmean(h + 1)
                        gate_p1(h + 1, 0)
                    elif g == 2:
                        gate_p2(h + 1, 0)
                    elif g == 3:
                        gate_p1(h + 1, 1)
                    elif g == 5:
                        gate_p2(h + 1, 1)
        while pending_tail:
            pending_tail.pop(0)()
        P.barrier()
    if stage == "D":
        stop_here()
        return

    wcq_d = dram_in("w_cq", [D, 512]); wckv_d = dram_in("w_ckv", [D, D]); wco_d = dram_in("w_co", [512, D])
    wcq_b = sb("wcq_b", [128, 8, 512], BF16); wckv_b = sb("wckv_b", [128, 8, D], BF16); wco_b = sb("wco_b", [128, 4, D], BF16)
    P.dma("pool", lambda e: e.dma_start(wcq_b[:], wcq_d.rearrange("(k p) n -> p k n", p=128)), "pfF0", [], ["wcq_b"])
    for hh_ in range(2):
        P.dma("pool", lambda e, hh_=hh_: e.dma_start(wckv_b[:, hh_ * 4:(hh_ + 1) * 4, :],
              wckv_d[hh_ * 512:(hh_ + 1) * 512, :].rearrange("(k p) n -> p k n", p=128)), "pfF1", [], ["wckv_b"])
    P.dma("pool", lambda e: e.dma_start(wco_b[:], wco_d.rearrange("(k p) n -> p k n", p=128)), "pfF2", [], ["wco_b"])
    gcross_d = dram_in("g_cross_t", [128, 8])
    X1 = dram_scr("X1", [TOWN, D], F32)
    H2T = dram_scr("H2T", [8, 128, TOWN])

    def norm_T(xtile, xkey, gt, dstT, dkey, tcs, pes_t, pbank_t):
        junk, stat, xn_ = pes_t
        P.act("act", junk[:], xtile, AF.Square, [xkey], ["junk", "nstat"], accum=stat[:, 0:1])
        P.ts("dve", stat[:, 1:2], stat[:, 0:1], 1.0 / D, EPS, ALU.mult, ALU.add, ["nstat"], ["nstatb"])
        P.act("act", stat[:, 2:3], stat[:, 1:2], AF.Ln, ["nstatb"], ["nstatc"])
        P.act("act", stat[:, 3:4], stat[:, 2:3], AF.Exp, ["nstatc"], ["nstatd"], scale=-0.5)
        P.ts("dve", xn_[:], xtile, stat[:, 3:4], None, ALU.mult, None, [xkey, "nstatd"], ["nxn"])
        pt = ps[pbank_t][:].bitcast(BF16)
        for k in range(8):
            P.tr(pt[:, k * 128:(k + 1) * 128], xn_[:, k * 128:(k + 1) * 128], ident_b[:], ["nxn", "ident_b"], [PSK[pbank_t]])
        P.tt("dve", dstT[:, :, tcs], pt.rearrange("p (k t) -> p k t", k=8), bcast(gt[:].unsqueeze(2), [128, 8, 128]), ALU.mult,
             [PSK[pbank_t], "gT"], [dkey])

    with ExitStack() as pes:
        def psb(name, shape, dt=F32):
            return pes.enter_context(nc.sbuf_tensor(uniq(name), list(shape), dt))
        gT = psb("gT", [128, 8])
        P.ld(gT[:], gcross_d, "e0", [], ["gT"])
        ogT = [psb(f"ogT{i}", [128, 4, 512], BF16) for i in range(2)]
        omTt = [psb(f"omTt{i}", [128, 4, 512], BF16) for i in range(2)]
        zgT = [psb(f"zgT{i}", [128, 8, 512], BF16) for i in range(2)]
        zmT = [psb(f"zmT{i}", [128, 8, 512], BF16) for i in range(2)]
        mgT = psb("mgT", [128, 8, 512], BF16)
        sg = psb("sg", [128, 512]); sm = psb("sm", [128, 512]); m1 = psb("m1", [128, 512]); m2 = psb("m2", [128, 512])
        x1t = [psb(f"x1t{i}", [128, D]) for i in range(4)]
        junk = psb("ejunk", [128, D], BF16); stat4 = psb("estat4", [128, 4, 4]); xn2 = [psb(f"exn{i}", [128, D], BF16) for i in range(2)]
        h2Tg = [psb(f"h2Tg{i}", [128, 8, 512], BF16) for i in range(2)]
        x_v = x_ext.rearrange("(n p) d -> n p d", p=128)
        X1v = X1.rearrange("(n p) d -> n p d", p=128)
        for g in range(8):
            s = g % 2
            osl = slice(g * 512, (g + 1) * 512)
            P.ld(ogT[s][:], OGT[:, :, osl].rearrange("c p t -> p c t"), f"ogT{s}", ["OGT"], [f"ogT{s}"])
            P.ld(omTt[s][:], OMT[:, :, osl].rearrange("c p t -> p c t"), f"omTt{s}", ["OMT"], [f"omTt{s}"])
            P.ld(zgT[s][:], ZGT[:, :, osl].rearrange("c p t -> p c t"), f"zgT{s}", ["ZGT"], [f"zgT{s}"])
            P.ld(zmT[s][:], ZMT[:, :, osl].rearrange("c p t -> p c t"), f"zmT{s}", ["ZMT"], [f"zmT{s}"])
            for n in range(8):
                ba, bb = (2 * n) % 4, (2 * n + 1) % 4
                nsl = slice(n * 128, (n + 1) * 128)
                for k in range(4):
                    P.mm(ps[ba][:, :], wpg_b[:, k, nsl], ogT[s][:, k, :], k == 0, k == 3, ["wpg_b", f"ogT{s}"], [PSK[ba]])
                for k in range(4):
                    P.mm(ps[bb][:, :], wpm_b[:, k, nsl], omTt[s][:, k, :], k == 0, k == 3, ["wpm_b", f"omTt{s}"], [PSK[bb]])
                P.act("act", sg[:], zgT[s][:, n, :], AF.Sigmoid, [f"zgT{s}"], ["sg"])
                P.act("act", sm[:], zmT[s][:, n, :], AF.Sigmoid, [f"zmT{s}"], ["sm"])
                P.tt("dve", m1[:], ps[ba][:, :], sg[:], ALU.mult, [PSK[ba], "sg"], ["m1"])
                P.tt("dve", m2[:], ps[bb][:, :], sm[:], ALU.mult, [PSK[bb], "sm"], ["m2"])
                P.tt("pool", mgT[:, n, :], m1[:], m2[:], ALU.add, ["m1", "m2"], ["mgT"])
            for t in range(4):
                i = g * 4 + t
                tcs = slice(t * 128, (t + 1) * 128)
                P.ld(x1t[t][:], x_v[32 + i], f"x1l{t}", [], [f"x1t{t}"])
                for hf in range(2):
                    b = 4 + hf
                    for k in range(8):
                        P.mm(ps[b][:, :], mgT[:, k, tcs], wout_b[:, k, hf * 512:(hf + 1) * 512], k == 0, k == 7, ["mgT", "wout_b"], [PSK[b]])
                    P.tt("dve", x1t[t][:, hf * 512:(hf + 1) * 512], ps[b][:, :], x1t[t][:, hf * 512:(hf + 1) * 512], ALU.add,
                         [PSK[b], f"x1t{t}"], [f"x1t{t}"])
                P.st(X1v[i], x1t[t][:], f"x1t{t}", [f"x1t{t}"], ["X1"])
                P.act("act", junk[:], x1t[t][:], AF.Square, [f"x1t{t}"], ["junk", f"es{t}a"], accum=stat4[:, t, 0:1])
                P.ts("dve", stat4[:, t, 1:2], stat4[:, t, 0:1], 1.0 / D, EPS, ALU.mult, ALU.add, [f"es{t}a"], [f"es{t}b"])
                P.act("act", stat4[:, t, 2:3], stat4[:, t, 1:2], AF.Ln, [f"es{t}b"], [f"es{t}c"])
                P.act("act", stat4[:, t, 3:4], stat4[:, t, 2:3], AF.Exp, [f"es{t}c"], [f"es{t}d"], scale=-0.5)
            for t in range(4):
                tcs = slice(t * 128, (t + 1) * 128)
                xb = t % 2
                P.ts("dve", xn2[xb][:], x1t[t][:], stat4[:, t, 3:4], None, ALU.mult, None, [f"x1t{t}", f"es{t}d"], [f"exn{xb}"])
                pb = 6 + xb
                pt = ps[pb][:].bitcast(BF16)
                for k in range(8):
                    P.tr(pt[:, k * 128:(k + 1) * 128], xn2[xb][:, k * 128:(k + 1) * 128], ident_b[:], [f"exn{xb}", "ident_b"], [PSK[pb]])
                P.tt(["dve", "pool"][0], h2Tg[s][:, :, tcs], pt.rearrange("p (k t) -> p k t", k=8), bcast(gT[:].unsqueeze(2), [128, 8, 128]), ALU.mult,
                     [PSK[pb], "gT"], [f"h2Tg{s}"])
            P.st(H2T[:, :, osl].rearrange("c p t -> p c t"), h2Tg[s][:], f"h2Tg{s}", [f"h2Tg{s}"], ["H2T"])
        P.barrier()
    if stage == "E":
        stop_here()
        return

    mem_d = dram_in("mem", [256, D]); gmem_d = dram_in("g_mem_t", [128, 8])
    gmoe_d = dram_in("g_moe_rep", [128, D]); wr_d = dram_in("w_router", [D, 36]); br_d = dram_in("b_router_rep", [128, 36])
    slt_d = dram_in("slt", [128, 128]); eoff_d = dram_in("eoff_rep", [128, 32])
    X2 = dram_scr("X2", [TOWN, D], F32)
    destall = sb("destall", [128, 32, 2], I32)
    wall = sb("wall", [128, 32, 2])
    with ExitStack() as pes:
        def psb(name, shape, dt=F32):
            return pes.enter_context(nc.sbuf_tensor(uniq(name), list(shape), dt))
        wr_f = psb("wr_f", [128, 8, 36]); wr_b = psb("wr_b", [128, 8, 36], BF16)
        P.ld(wr_f[:], wr_d.rearrange("(k p) n -> p k n", p=128), "f0", [], ["wr_f"])
        P.copy("dve", wr_b[:], wr_f[:], ["wr_f"], ["wr_b"])
        gT = psb("fgT", [128, 8]); P.ld(gT[:], gmem_d, "f1", [], ["gT"])
        gmoe = psb("gmoe", [128, D]); P.ld(gmoe[:], gmoe_d, "f2", [], ["gmoe"])
        brr = psb("brr", [128, 36]); P.ld(brr[:], br_d, "f3", [], ["brr"])
        slt_f = psb("slt_f", [128, 128]); slt_b = psb("slt_b", [128, 128], BF16)
        P.ld(slt_f[:], slt_d, "f4", [], ["slt_f"]); P.copy("dve", slt_b[:], slt_f[:], ["slt_f"], ["slt_b"])
        eoff = psb("eoff", [128, 32]); P.ld(eoff[:], eoff_d, "f5", [], ["eoff"])
        ones_b = psb("ones_b", [128, 128], BF16)
        P.op("dve", lambda e: e.memset(ones_b[:], 1.0), [], ["ones_b"])
        base = psb("base", [128, 32])
        P.op("dve", lambda e: e.memset(base[:], 0.0), [], ["base"])
        junk = psb("fjunk", [128, D], BF16); stat = psb("fstat", [128, 4]); xn_ = psb("fxn", [128, D], BF16)
        memt = psb("memt", [128, D]); memT = psb("memT", [128, 8, 256], BF16)
        mem_v = mem_d.rearrange("(n p) d -> n p d", p=128)
        for mt in range(2):
            P.ld(memt[:], mem_v[mt], "f6", [], ["memt"])
            norm_T(memt[:], "memt", gT, memT, "memT", slice(mt * 128, (mt + 1) * 128), (junk, stat, xn_), 7)
        KTm = psb("KTm", [128, 4, 256], BF16); Vm = psb("Vm", [128, 2, 512], BF16)
        for h in range(4):
            for k in range(8):
                P.mm(ps[0][:, 0:256], wckv_b[:, k, h * 128:(h + 1) * 128], memT[:, k, :], k == 0, k == 7, ["wckv_b", "memT"], ["ps0"])
            P.copy("dve", KTm[:, h, :], ps[0][:, 0:256], ["ps0"], ["KTm"])
        for mt in range(2):
            for k in range(8):
                P.mm(ps[1][:, :], memT[:, k, mt * 128:(mt + 1) * 128], wckv_b[:, k, 512:1024], k == 0, k == 7, ["wckv_b", "memT"], ["ps1"])
            P.copy("dve", Vm[:, mt, :], ps[1][:, :], ["ps1"], ["Vm"])
        h2T = [psb(f"h2T{i}", [128, 8, 512], BF16) for i in range(2)]
        qTh2 = [psb(f"qTh{i}", [128, 512], BF16) for i in range(2)]
        PT = [psb(f"fPT{i}", [128, 512], BF16) for i in range(2)]
        frec = psb("frec", [128, 512]); oT = psb("oT", [128, 4, 512], BF16)
        x1t = [psb(f"fx1t{i}", [128, D]) for i in range(4)]
        x2t = [psb(f"fx2t{i}", [128, D]) for i in range(4)]
        stat4 = psb("stat4", [128, 4, 4])
        h3g = [psb(f"h3g{i}", [128, 4, D], BF16) for i in range(2)]
        h3T = [psb(f"h3T{i}", [128, 8, 128], BF16) for i in range(2)]
        lg = psb("lg", [128, 144]); sm8 = psb("sm8", [128, 36]); gsel = psb("gsel", [128, 16]); pen = psb("pen", [128, 16])
        lem = psb("lem", [128, 128]); lem2 = psb("lem2", [128, 128]); oh1 = psb("oh1", [128, 128]); oh2 = psb("oh2", [128, 128])
        A_b = psb("A_b", [128, 128], BF16); rank = psb("rank", [128, 128]); tmp32 = psb("tmp32", [128, 128]); dst2 = psb("dst2", [128, 8])
        gej = psb("gej", [128, 16])
        X1v = X1.rearrange("(n p) d -> n p d", p=128)
        X2v = X2.rearrange("(n p) d -> n p d", p=128)
        pti = 0
        for g in range(8):
            s = g % 2
            osl = slice(g * 512, (g + 1) * 512)
            P.ld(h2T[s][:], H2T[:, :, osl].rearrange("c p t -> p c t"), f"h2T{s}", ["H2T"], [f"h2T{s}"])
            def f_qproj(h):
                qb = [0, 7][h % 2]
                for k in range(8):
                    P.mm(ps[qb][:, :], wcq_b[:, k, h * 128:(h + 1) * 128], h2T[s][:, k, :], k == 0, k == 7, ["wcq_b", f"h2T{s}"], [PSK[qb]])
                P.copy("dve", qTh2[h % 2][:], ps[qb][:, :], [PSK[qb]], [f"qTh{h % 2}"])

            f_qproj(0)
            for h in range(4):
                if h + 1 < 4:
                    f_qproj(h + 1)
                ob, db_ = ((3, 4), (5, 6))[h % 2]
                for mt in range(2):
                    b = 1 + mt
                    P.mm(ps[b][:, :], KTm[:, h, mt * 128:(mt + 1) * 128], qTh2[h % 2][:], True, True, ["KTm", f"qTh{h % 2}"], [PSK[b]])
                    p = pti % 2
                    pti += 1
                    P.act("act", PT[p][:], ps[b][:, :], AF.Exp, [PSK[b]], [f"fPT{p}"], scale=128.0 ** -0.5)
                    P.mm(ps[ob][:, :], Vm[:, mt, h * 128:(h + 1) * 128], PT[p][:], mt == 0, mt == 1, ["Vm", f"fPT{p}"], [PSK[ob]])
                    P.mm(ps[db_][:, :], ones_b[:], PT[p][:], mt == 0, mt == 1, ["ones_b", f"fPT{p}"], [PSK[db_]])
                P.op("dve", lambda e, db_=db_: e.reciprocal(frec[:], ps[db_][:, :]), [PSK[db_]], ["frec"])
                P.tt("dve", oT[:, h, :], ps[ob][:, :], frec[:], ALU.mult, [PSK[ob], "frec"], ["oT"])
            gs_ = g % 2
            h3k = f"h3g{gs_}"
            for t in range(4):
                i = g * 4 + t
                xs_ = t
                tcs = slice(t * 128, (t + 1) * 128)
                P.ld(x1t[xs_][:], X1v[i], f"fx1t{xs_}", ["X1"], [f"fx1t{xs_}"])
                for hf in range(2):
                    b = 5 + hf
                    for k in range(4):
                        P.mm(ps[b][:, :], oT[:, k, tcs], wco_b[:, k, hf * 512:(hf + 1) * 512], k == 0, k == 3, ["oT", "wco_b"], [PSK[b]])
                    P.tt("dve", x2t[xs_][:, hf * 512:(hf + 1) * 512], ps[b][:, :], x1t[xs_][:, hf * 512:(hf + 1) * 512], ALU.add,
                         [PSK[b], f"fx1t{xs_}"], [f"fx2t{xs_}"])
                P.st(X2v[i], x2t[xs_][:], f"fx2t{xs_}", [f"fx2t{xs_}"], ["X2"])
                xk = f"fx2t{xs_}"
                P.act("act", junk[:], x2t[xs_][:], AF.Square, [xk], ["junk", f"ns{t}a"], accum=stat4[:, t, 0:1])
                P.ts("dve", stat4[:, t, 1:2], stat4[:, t, 0:1], 1.0 / D, EPS, ALU.mult, ALU.add, [f"ns{t}a"], [f"ns{t}b"])
                P.act("act", stat4[:, t, 2:3], stat4[:, t, 1:2], AF.Ln, [f"ns{t}b"], [f"ns{t}c"])
                P.act("act", stat4[:, t, 3:4], stat4[:, t, 2:3], AF.Exp, [f"ns{t}c"], [f"ns{t}d"], scale=-0.5)
            def fB_tr(t):
                xs_ = t
                xk = f"fx2t{xs_}"
                hb = t % 2
                P.stt(h3g[gs_][:, t, :], x2t[xs_][:], stat4[:, t, 3:4], gmoe[:], ALU.mult, ALU.mult, [xk, f"ns{t}d", "gmoe"], [h3k])
                pt = ps[7][:].bitcast(BF16) if hb == 0 else ps[3][:].bitcast(BF16)
                pk = "ps7" if hb == 0 else "ps3"
                for k in range(8):
                    P.tr(pt[:, k * 128:(k + 1) * 128], h3g[gs_][:, t, k * 128:(k + 1) * 128], ident_b[:], [h3k, "ident_b"], [pk])
                P.copy(["act", "dve"][hb], h3T[hb][:].rearrange("p k t -> p (k t)"), pt, [pk], [f"h3T{hb}"])

            def fB_router(t):
                hb = t % 2
                for k in range(8):
                    P.mm(ps[0][:, t * 36:(t + 1) * 36], h3T[hb][:, k, :], wr_b[:, k, :], k == 0, k == 7, [f"h3T{hb}", "wr_b"], ["ps0"])

            fB_tr(0); fB_tr(1); fB_router(0); fB_tr(2); fB_router(1); fB_tr(3); fB_router(2); fB_router(3)
            i0 = g * 4
            gs_ = g % 2
            h3k = f"h3g{gs_}"
            lg3 = lg[:].rearrange("p (t n) -> p t n", t=4)
            P.tt("dve", lg3, ps[0][:, 0:144].rearrange("p (t n) -> p t n", t=4), bcast(brr[:].unsqueeze(1), [128, 4, 36]), ALU.add,
                 ["ps0", "brr"], ["lg"])
            lgg = lg3[:, :, 0:4]
            P.op("dve", lambda e, lgg=lgg: e.tensor_reduce(sm8[:, 0:4], lgg, AX.X, ALU.max), ["lg"], ["sm_a"])
            gsel3 = gsel[:].rearrange("p (t n) -> p t n", t=4)
            P.tt("dve", gsel3, lgg, bcast(sm8[:, 0:4].unsqueeze(2), [128, 4, 4]), ALU.is_ge, ["lg", "sm_a"], ["gsel"])
            gej3 = gej[:].rearrange("p (t n) -> p t n", t=4)
            P.tt("dve", gej3, lgg, bcast(sm8[:, 0:4].unsqueeze(2), [128, 4, 4]), ALU.subtract, ["lg", "sm_a"], ["gej"])
            P.act("act", gej[:], gej[:], AF.Exp, ["gej"], ["gej"])
            P.op("dve", lambda e, gej3=gej3: e.tensor_reduce(sm8[:, 4:8], gej3, AX.X, ALU.add), ["gej"], ["sm_c"])
            P.op("dve", lambda e: e.reciprocal(sm8[:, 8:12], sm8[:, 4:8]), ["sm_c"], ["sm_d"])
            P.ts("dve", pen[:], gsel[:], 1e30, -1e30, ALU.mult, ALU.add, ["gsel"], ["pen"])
            lem4 = lem[:].rearrange("p (t g e) -> p t g e", t=4, g=4)
            P.tt("dve", lem4, lg3[:, :, 4:36].rearrange("p t (g e) -> p t g e", g=4),
                 bcast(pen[:].rearrange("p (t g) -> p t g", t=4).unsqueeze(3), [128, 4, 4, 8]), ALU.add, ["lg", "pen"], ["lem"])
            lem3 = lem[:].rearrange("p (t n) -> p t n", t=4)
            lem23 = lem2[:].rearrange("p (t n) -> p t n", t=4)
            oh13 = oh1[:].rearrange("p (t n) -> p t n", t=4)
            oh23 = oh2[:].rearrange("p (t n) -> p t n", t=4)
            P.op("dve", lambda e, lem3=lem3: e.tensor_reduce(sm8[:, 12:16], lem3, AX.X, ALU.max), ["lem"], ["sm_m1"])
            P.tt("dve", oh13, lem3, bcast(sm8[:, 12:16].unsqueeze(2), [128, 4, 32]), ALU.is_equal, ["lem", "sm_m1"], ["oh1"])
            P.stt(lem2[:], oh1[:], -1e30, lem[:], ALU.mult, ALU.add, ["oh1", "lem"], ["lem2"])
            P.op("dve", lambda e, lem23=lem23: e.tensor_reduce(sm8[:, 16:20], lem23, AX.X, ALU.max), ["lem2"], ["sm_m2"])
            P.tt("dve", oh23, lem23, bcast(sm8[:, 16:20].unsqueeze(2), [128, 4, 32]), ALU.is_equal, ["lem2", "sm_m2"], ["oh2"])
            P.tt("dve", sm8[:, 20:24], sm8[:, 16:20], sm8[:, 12:16], ALU.subtract, ["sm_m1", "sm_m2"], ["sm_e"])
            P.act("act", sm8[:, 24:28], sm8[:, 20:24], AF.Exp, ["sm_e"], ["sm_f"])
            P.ts("dve", sm8[:, 28:32], sm8[:, 24:28], 1.0, None, ALU.add, None, ["sm_f"], ["sm_g"])
            P.op("dve", lambda e: e.reciprocal(sm8[:, 32:36], sm8[:, 28:32]), ["sm_g"], ["sm_h"])
            P.tt("dve", wall[:, i0:i0 + 4, 0], sm8[:, 8:12], sm8[:, 32:36], ALU.mult, ["sm_d", "sm_h"], ["wall"])
            P.tt("dve", wall[:, i0:i0 + 4, 1], wall[:, i0:i0 + 4, 0], sm8[:, 24:28], ALU.mult, ["wall", "sm_f"], ["wall"])
            A3 = A_b[:].rearrange("p (t n) -> p t n", t=4)
            P.tt("dve", A_b[:], oh1[:], oh2[:], ALU.add, ["oh1", "oh2"], ["A_b"])
            for t in range(4):
                P.mm(ps[1][:, t * 32:(t + 1) * 32], slt_b[:], A3[:, t, :], True, t == 0, ["slt_b", "A_b"], ["ps1"])
                for t2 in range(t):
                    P.mm(ps[1][:, t * 32:(t + 1) * 32], ones_b[:], A3[:, t2, :], False, t2 == t - 1, ["ones_b", "A_b"], ["ps1"])
            rank3 = rank[:].rearrange("p (t n) -> p t n", t=4)
            P.tt("dve", rank3, ps[1][:, 0:128].rearrange("p (t n) -> p t n", t=4), bcast(base[:].unsqueeze(1), [128, 4, 32]), ALU.add,
                 ["ps1", "base"], ["rank"])
            for t in range(4):
                P.mm(ps[2][:, 0:32], ones_b[:], A3[:, t, :], t == 0, t == 3, ["ones_b", "A_b"], ["ps2"])
            P.tt("dve", base[:], base[:], ps[2][:, 0:32], ALU.add, ["base", "ps2"], ["base"])
            P.ts("dve", rank[:], rank[:], float(CAP - 1), None, ALU.min, None, ["rank"], ["rank"])
            P.tt("dve", rank3, rank3, bcast(eoff[:].unsqueeze(1), [128, 4, 32]), ALU.add, ["rank", "eoff"], ["rank"])
            P.tt("dve", tmp32[:], rank[:], oh1[:], ALU.mult, ["rank", "oh1"], ["tmp32"])
            P.op("dve", lambda e: e.tensor_reduce(dst2[:, 0:4], tmp32[:].rearrange("p (t n) -> p t n", t=4), AX.X, ALU.add), ["tmp32"], ["dst2"])
            P.tt("dve", tmp32[:], rank[:], oh2[:], ALU.mult, ["rank", "oh2"], ["tmp32"])
            P.op("dve", lambda e: e.tensor_reduce(dst2[:, 4:8], tmp32[:].rearrange("p (t n) -> p t n", t=4), AX.X, ALU.add), ["tmp32"], ["dst2"])
            P.copy("dve", destall[:, i0:i0 + 4, :], dst2[:].rearrange("p (j t) -> p t j", j=2), ["dst2"], ["destall"])
            for t in range(4):
                for jj in range(2):
                    P.dma("pool", lambda e, i=i0 + t, jj=jj, t=t, gs_=gs_: e.indirect_dma_start(
                        out=XS[:, :], out_offset=bass.IndirectOffsetOnAxis(ap=destall[:, i, jj:jj + 1], axis=0),
                        in_=h3g[gs_][:, t, :], in_offset=None), f"sc{gs_}{t}{jj}", [h3k, "destall"], ["XS"])
        if dbg:
            DEST = nc.dram_tensor("DEST", [128, 64], I32, kind="ExternalOutput").ap()
            WALL = nc.dram_tensor("WALL", [128, 64], F32, kind="ExternalOutput").ap()
            P.st(DEST, destall[:].rearrange("p a b -> p (a b)"), "dd0", ["destall"], ["DEST"])
            P.st(WALL, wall[:].rearrange("p a b -> p (a b)"), "dd1", ["wall"], ["WALL"])
        P.barrier()
    if stage == "F":
        stop_here()
        return

    wg_d = dram_in("w_exp_gate", [NEXP, D, 512]); wu_d = dram_in("w_exp_up", [NEXP, D, 512]); wd_d = dram_in("w_exp_down", [NEXP, 512, D])
    YS = dram_scr("YS", [NROWS, D], F32)
    with ExitStack() as pes:
        def psb(name, shape, dt=F32):
            return pes.enter_context(nc.sbuf_tensor(uniq(name), list(shape), dt))
        wg_b = [psb(f"wg_b{i}", [128, 8, 512], BF16) for i in range(2)]
        wu_b = [psb(f"wu_b{i}", [128, 8, 512], BF16) for i in range(2)]
        wd_b = [psb(f"wd_b{i}", [128, 4, D], BF16) for i in range(2)]
        xs_t = [psb(f"xs_t{i}", [128, 3, D], BF16) for i in range(2)]
        xsT = [psb(f"xsT{i}", [128, 8, CAP], BF16) for i in range(2)]
        sgl = psb("sgl", [128, CAP]); hidT = psb("hidT", [128, 4, CAP], BF16)
        ysb = [psb(f"ysb{i}", [128, D]) for i in range(2)]
        ci = [0]
        yi = 0
        NBLK = CAP // 128
        def g_load_pieces(e_):
            s = e_ % 2
            pcs = []

            def castdma(dst, src, dkey):
                return lambda: P.dma("pool", lambda e: e.dma_start(dst, src), dkey + "q", [], [dkey])
            for half in range(2):
                pcs.append(castdma(wg_b[s][:, half * 4:(half + 1) * 4, :],
                           wg_d[e_, half * 512:(half + 1) * 512, :].rearrange("(k p) n -> p k n", p=128), f"wg_b{s}"))
                pcs.append(castdma(wu_b[s][:, half * 4:(half + 1) * 4, :],
                           wu_d[e_, half * 512:(half + 1) * 512, :].rearrange("(k p) n -> p k n", p=128), f"wu_b{s}"))
            for half in range(2):
                pcs.append(castdma(wd_b[s][:, half * 2:(half + 1) * 2, :],
                           wd_d[e_, half * 256:(half + 1) * 256, :].rearrange("(k p) n -> p k n", p=128), f"wd_b{s}"))
            pcs.append(lambda: P.ld(xs_t[s][:], XS[e_ * CAP:(e_ + 1) * CAP, :].rearrange("(t p) d -> p t d", p=128), f"xs_t{s}", ["XS"], [f"xs_t{s}"]))
            return pcs

        def g_transposes(e2, blocks):
            s2 = e2 % 2
            for t in blocks:
                pt = ps[6 + (t % 2)][:].bitcast(BF16)
                for k in range(8):
                    P.tr(pt[:, k * 128:(k + 1) * 128], xs_t[s2][:, t, k * 128:(k + 1) * 128], ident_b[:], [f"xs_t{s2}", "ident_b"], [PSK[6 + (t % 2)]])
                P.copy(["act", "dve"][t % 2], xsT[s2][:, :, t * 128:(t + 1) * 128], pt.rearrange("p (k c) -> p k c", k=8),
                       [PSK[6 + (t % 2)]], [f"xsT{s2}"])

        for pc in g_load_pieces(0):
            pc()
        g_transposes(0, range(NBLK))
        for e_ in range(NEXP):
            s = e_ % 2
            pieces = g_load_pieces(e_ + 1) if e_ + 1 < NEXP else []
            while pieces:
                pieces.pop()()
            for f in range(4):
                bg, bu = (2 * f) % 4, (2 * f + 1) % 4
                fsl = slice(f * 128, (f + 1) * 128)
                for k in range(8):
                    P.mm(ps[bg][:, 0:CAP], wg_b[s][:, k, fsl], xsT[s][:, k, :], k == 0, k == 7, [f"wg_b{s}", f"xsT{s}"], [PSK[bg]])
                for k in range(8):
                    P.mm(ps[bu][:, 0:CAP], wu_b[s][:, k, fsl], xsT[s][:, k, :], k == 0, k == 7, [f"wu_b{s}", f"xsT{s}"], [PSK[bu]])
                P.act("act", sgl[:], ps[bg][:, 0:CAP], AF.Silu, [PSK[bg]], ["sgl"])
                P.tt("dve", hidT[:, f, :], ps[bu][:, 0:CAP], sgl[:], ALU.mult, [PSK[bu], "sgl"], ["hidT"])
                if pieces:
                    pieces.pop(0)()
            for t in range(NBLK):
                ys = yi % 2
                yi += 1
                for hf in range(2):
                    b = (4, 5, 0, 1, 2, 3)[t * 2 + hf]
                    for f in range(4):
                        P.mm(ps[b][:, :], hidT[:, f, t * 128:(t + 1) * 128], wd_b[s][:, f, hf * 512:(hf + 1) * 512], f == 0, f == 3,
                             ["hidT", f"wd_b{s}"], [PSK[b]])
                    P.copy(["act", "dve"][hf], ysb[ys][:, hf * 512:(hf + 1) * 512], ps[b][:, :], [PSK[b]], [f"ysb{ys}"])
                r0 = e_ * CAP + t * 128
                P.st(YS[r0:r0 + 128, :], ysb[ys][:], f"ysb{ys}", [f"ysb{ys}"], ["YS"], queue="sp")
                if pieces:
                    pieces.pop(0)()
                if e_ + 1 < NEXP:
                    g_transposes(e_ + 1, [t])
        P.barrier()
    if stage == "G":
        stop_here()
        return

    gfin_d = dram_in("g_final_rep", [128, D])
    with ExitStack() as pes:
        def psb(name, shape, dt=F32):
            return pes.enter_context(nc.sbuf_tensor(uniq(name), list(shape), dt))
        gfin = psb("gfin", [128, D]); P.ld(gfin[:], gfin_d, "h0", [], ["gfin"])
        NS = 4
        y1 = [psb(f"y1_{i}", [128, D]) for i in range(NS)]
        y2 = [psb(f"y2_{i}", [128, D]) for i in range(NS)]
        x2t = [psb(f"hx2t{i}", [128, D]) for i in range(NS)]
        x3 = [psb(f"x3_{i}", [128, D]) for i in range(2)]
        ot = [psb(f"ot{i}", [128, D]) for i in range(2)]
        junk = psb("hjunk", [128, D], BF16); stat = [psb(f"hstat{i}", [128, 4]) for i in range(2)]
        X2v = X2.rearrange("(n p) d -> n p d", p=128)
        outv = out_d.rearrange("(n p) d -> n p d", p=128)
        def h_loads(i):
            s = i % NS
            P.ld(x2t[s][:], X2v[i], f"hx2t{s}", ["X2"], [f"hx2t{s}"])
            for jj, yb in enumerate((y1, y2)):
                P.dma("pool", lambda e, i=i, jj=jj, yb=yb, s=s: e.indirect_dma_start(
                    out=yb[s][:, :], out_offset=None, in_=YS[:, :],
                    in_offset=bass.IndirectOffsetOnAxis(ap=destall[:, i, jj:jj + 1], axis=0)),
                    f"ga{s}{jj}", ["YS", "destall"], [f"y{jj}_{s}"])

        def h_A(i):
            s = i % NS
            s2 = i % 2
            P.stt(x3[s2][:], y1[s][:], wall[:, i, 0:1], x2t[s][:], ALU.mult, ALU.add, [f"y0_{s}", "wall", f"hx2t{s}"], [f"x3_{s2}"])
            P.stt(x3[s2][:], y2[s][:], wall[:, i, 1:2], x3[s2][:], ALU.mult, ALU.add, [f"y1_{s}", "wall", f"x3_{s2}"], [f"x3_{s2}"])
            P.act("act", junk[:], x3[s2][:], AF.Square, [f"x3_{s2}"], ["junk", f"hs{s2}a"], accum=stat[s2][:, 0:1])

        def h_B(i):
            s2 = i % 2
            P.ts("dve", stat[s2][:, 1:2], stat[s2][:, 0:1], 1.0 / D, EPS, ALU.mult, ALU.add, [f"hs{s2}a"], [f"hs{s2}b"])
            P.act("act", stat[s2][:, 2:3], stat[s2][:, 1:2], AF.Ln, [f"hs{s2}b"], [f"hs{s2}c"])
            P.act("act", stat[s2][:, 3:4], stat[s2][:, 2:3], AF.Exp, [f"hs{s2}c"], [f"hs{s2}d"], scale=-0.5)
            P.stt(ot[s2][:], x3[s2][:], stat[s2][:, 3:4], gfin[:], ALU.mult, ALU.mult, [f"x3_{s2}", f"hs{s2}d", "gfin"], [f"ot{s2}"])
            P.st(outv[i], ot[s2][:], f"ot{s2}", [f"ot{s2}"], ["out"], is_out=True, queue="sp")

        for i in range(NS - 1):
            h_loads(i)
        h_A(0)
        for i in range(32):
            if i + NS - 1 < 32:
                h_loads(i + NS - 1)
            if i + 1 < 32:
                h_A(i + 1)
            h_B(i)
        P.barrier()
    P.finish()


DBG_OUT = {"QGT", "KGT", "AT", "QMT", "KMT", "ZGT", "ZMT", "VG", "KG", "RG", "VM", "OGT", "OMT", "X1", "H2T", "X2", "XS", "YS"}


def t5_bucket_np(dist):
    n = np.maximum(dist, 0)
    nf = np.maximum(n, 1).astype(np.float32)
    large = 16 + (np.log(nf / np.float32(16)) / np.float32(np.log(8.0)) * np.float32(16)).astype(np.int32)
    large = np.minimum(large, 31)
    return np.where(n < 16, n, large)


def host_consts(inputs):
    f = lambda k: np.asarray(inputs[k], np.float32)
    bf = ml_dtypes.bfloat16
    c = {}
    c["w_in"] = np.ascontiguousarray(f("w_in")[0])
    c["g_mix_t"] = np.ascontiguousarray(f("g_mix")[0].reshape(8, 128).T)
    c["g_cross_t"] = np.ascontiguousarray(f("g_cross")[0].reshape(8, 128).T)
    c["g_mem_t"] = np.ascontiguousarray(f("g_mem").reshape(8, 128).T)
    c["ident"] = np.eye(128, dtype=np.float32)
    c["w_alpha_up"] = np.ascontiguousarray(f("w_alpha_up")[0])
    c["b_alpha"] = np.ascontiguousarray(f("b_alpha")[0].reshape(1, 256))
    s_ = np.arange(128)
    c["u2"] = ((s_[:, None] <= s_[None, :]) & ((s_[:, None] // 64) == (s_[None, :] // 64))).astype(np.float32)
    c["u3"] = ((s_[:, None] > s_[None, :]) & ((s_[:, None] // 64) == (s_[None, :] // 64))).astype(np.float32)
    s6 = np.arange(64)
    c["maskT"] = (s6[:, None] <= s6[None, :]).astype(np.float32)
    c["ggla_rep"] = np.ascontiguousarray(np.broadcast_to(f("g_gla_head")[0].reshape(1, 512), (64, 512)))
    c["ind"] = (np.arange(32)[:, None] == (np.arange(TEXT)[None, :] // 256)).astype(bf)
    rb = f("rel_bias")
    j = np.arange(6)[:, None, None]; kl = np.arange(128)[None, :, None]; q = np.arange(512)[None, None, :]
    kr = j * 128 + kl - 256
    kb = np.floor_divide(kr, 256); qb = q // 256
    dist = q - kr
    bucket = t5_bucket_np(dist)
    tb = np.zeros((8, 6, 128, 512), np.float32)
    own = (kb == qb); prev = (kb == qb - 1)
    for h in range(8):
        g_ = rb[bucket, h]
        t = np.where(own, np.where(dist >= 0, g_, np.float32(NEG)), np.where(prev, g_, np.float32(0)))
        tb[h] = t
    c["tb"] = np.ascontiguousarray(tb.transpose(0, 2, 1, 3)).astype(bf)
    c["cfar_rep"] = np.ascontiguousarray(np.broadcast_to(rb[31].reshape(1, 8), (128, 8)))
    k_ = np.arange(128)[:, None]; m_ = np.arange(64)[None, :]
    c["sh"] = (k_ == m_ + 64).astype(np.float32)
    c["w_proj_gla"] = np.ascontiguousarray(f("w_proj_gla")[0]); c["w_proj_moba"] = np.ascontiguousarray(f("w_proj_moba")[0])
    c["w_out"] = np.ascontiguousarray(f("w_out")[0])
    c["w_cq"] = np.ascontiguousarray(f("w_cq")[0]); c["w_ckv"] = np.ascontiguousarray(f("w_ckv")[0]); c["w_co"] = np.ascontiguousarray(f("w_co")[0])
    c["g_moe_rep"] = np.ascontiguousarray(np.broadcast_to(f("g_moe")[0].reshape(1, D), (128, D)))
    c["w_router"] = np.ascontiguousarray(np.concatenate([f("w_router_group")[0], f("w_router_expert")[0]], axis=1))
    c["b_router_rep"] = np.ascontiguousarray(np.broadcast_to(
        np.concatenate([f("b_router_group")[0], f("b_router_expert")[0]]).reshape(1, 36), (128, 36)))
    c["slt"] = (s_[:, None] < s_[None, :]).astype(np.float32)
    c["eoff_rep"] = np.ascontiguousarray(np.broadcast_to((np.arange(32) * CAP).astype(np.float32).reshape(1, 32), (128, 32)))
    c["w_exp_gate"] = np.ascontiguousarray(f("w_exp_gate")[0]); c["w_exp_up"] = np.ascontiguousarray(f("w_exp_up")[0])
    c["w_exp_down"] = np.ascontiguousarray(f("w_exp_down")[0])
    c["g_final_rep"] = np.ascontiguousarray(np.broadcast_to(f("g_final").reshape(1, D), (128, D)))
    return c


def host_inputs(inputs, c, consts=None, names=None):
    if consts is None:
        consts = host_consts(inputs)
    b, half = c // 2, c % 2
    x = np.asarray(inputs["x"], np.float32)
    xe = np.zeros((TEXT, D), np.float32)
    if half == 1:
        xe[:] = x[b]
    else:
        xe[TOWN:] = x[b, :TOWN]
    m = dict(consts)
    m["x_ext"] = xe
    m["mem"] = np.ascontiguousarray(np.asarray(inputs["mem"], np.float32)[b])
    qt = np.arange(32)[:, None]; n = np.arange(32)[None, :]
    ownb = 16 + qt // 2
    valid = (n < ownb) & ((half == 1) | (n >= 16))
    rep = lambda a: np.ascontiguousarray(np.broadcast_to(a.astype(np.float32)[None], (128, 32, 32)))
    m["gadd"] = rep(np.where(valid, 0.0, -1e30))
    m["pv01"] = rep(valid)
    m["bfix"] = rep(np.where(n == ownb, 0.0, NEG))
    m["cfm"] = rep(n < ownb - 1)
    if names is not None:
        m = {k: v for k, v in m.items() if k in names}
    return m


def kernel(**inputs):
    nc = build("full")
    consts = host_consts(inputs)
    in_maps = [host_inputs(inputs, c, consts, set(DECL_INPUTS)) for c in range(NCORES)]
    res = run_bass_kernel_spmd(nc, in_maps, core_ids=list(range(NCORES)))
    out = np.zeros((4, 8192, D), np.float32)
    for c in range(NCORES):
        b, half = c // 2, c % 2
        out[b, half * TOWN:(half + 1) * TOWN] = res.results[c]["out"]
    return out
```

```python
import numpy as np
import ml_dtypes
from contextlib import ExitStack
import concourse.bass as bass
import concourse.mybir as mybir
from concourse.bass_utils import run_bass_kernel_spmd

F32 = mybir.dt.float32
BF16 = mybir.dt.bfloat16
I32 = mybir.dt.int32
AF = mybir.ActivationFunctionType
ALU = mybir.AluOpType
AX = mybir.AxisListType

NCORES = 8
D = 1024
TOWN = 4096
TEXT = 8192
DIN = 5136
CAP = 384
NEXP = 32
NROWS = NEXP * CAP
EPS = 1e-6
NEG = -30000.0

C_QG, C_KG, C_VG, C_RG, C_A, C_QM, C_KM, C_VM, C_ZG, C_ZM = 0, 256, 512, 1024, 1536, 1552, 2064, 2576, 3088, 4112


class Prog:
    ENGS = ["pe", "act", "dve", "pool", "sp"]

    def __init__(self, nc, es):
        self.nc, self.es = nc, es
        self.q = {e: [] for e in self.ENGS}
        self.seq = {e: 0 for e in self.ENGS}
        self.cnt = {e: 0 for e in self.ENGS}
        self.sem = {}
        self.nsem = 0
        for e in self.ENGS:
            self._new_epoch(e)
        self.dma_pool = [es.enter_context(nc.semaphore(f"dq{i}")) for i in range(56)]
        self.dma_cnt = [0] * len(self.dma_pool)
        self.dma_key = {}
        self.dma_next = 0
        self.dma_nextq = [0, 0]
        self.last_w = {}
        self.readers = {}
        self.waited = {e: {} for e in self.ENGS}
        self.out_tokens = []

    def _new_epoch(self, e):
        self.sem[e] = self.es.enter_context(self.nc.semaphore(f"e_{e}_{self.nsem}"))
        self.nsem += 1
        self.cnt[e] = 0

    def _collect(self, reads, writes):
        deps = []
        for k in reads:
            t = self.last_w.get(k)
            if t is not None:
                deps.append(t)
        for k in writes:
            t = self.last_w.get(k)
            if t is not None:
                deps.append(t)
            deps.extend(self.readers.get(k, ()))
        return deps

    def _waits(self, eng, deps):
        best = {}
        for (sem, val, teng, tseq) in deps:
            if teng == eng and eng == "pe":
                continue
            key = id(sem)
            if self.waited[eng].get(key, 0) >= val:
                continue
            if key not in best or best[key][1] < val:
                best[key] = (sem, val)
        for key, (sem, val) in best.items():
            self.waited[eng][key] = val
        return list(best.values())

    def _record(self, tok, reads, writes):
        for k in writes:
            self.last_w[k] = tok
            self.readers[k] = []
        for k in reads:
            if k in writes:
                continue
            self.readers.setdefault(k, []).append(tok)

    def op(self, eng, fn, r=(), w=()):
        r, w = list(r), list(w)
        waits = self._waits(eng, self._collect(r, w))
        if self.cnt[eng] >= 20000:
            self._new_epoch(eng)
        self.cnt[eng] += 1
        self.seq[eng] += 1
        sem, val = self.sem[eng], self.cnt[eng]

        def emit(e, fn=fn, waits=waits, sem=sem):
            for (s, v) in waits:
                e.wait_ge(s, v)
            fn(e).then_inc(sem, 1)
        self.q[eng].append(emit)
        tok = (sem, val, eng, self.seq[eng])
        self._record(tok, r, w)
        return tok

    def dma(self, queue, fn, semkey, r=(), w=(), is_out=False):
        r, w = list(r), list(w)
        waits = self._waits(queue, self._collect(r, w))
        if semkey not in self.dma_key:
            half = len(self.dma_pool) // 2
            qi = 0 if queue == "pool" else 1
            self.dma_key[semkey] = (qi * half + self.dma_nextq[qi] % half, queue)
            self.dma_nextq[qi] += 1
        si, q0 = self.dma_key[semkey]
        assert q0 == queue, (semkey, q0, queue)
        self.dma_cnt[si] += 16
        sem, val = self.dma_pool[si], self.dma_cnt[si]

        def emit(e, fn=fn, waits=waits, sem=sem):
            for (s, v) in waits:
                e.wait_ge(s, v)
            fn(e).then_inc(sem, 16)
        self.q[queue].append(emit)
        tok = (sem, val, "dma", 0)
        self._record(tok, r, w)
        if is_out:
            self.out_tokens.append(tok)
        return tok

    def barrier(self):
        toks = []
        for t in self.last_w.values():
            toks.append(t)
        for l in self.readers.values():
            toks.extend(l)
        for e in self.ENGS:
            waits = self._waits(e, [(s, v, "x", 0) for (s, v, _, _) in toks])
            if waits:
                def emit(en, waits=waits):
                    for (s, v) in waits:
                        en.wait_ge(s, v)
                self.q[e].append(emit)
        self.last_w.clear()
        self.readers.clear()
        self.dma_key.clear()

    def finish(self):
        self.barrier()
        with self.nc.Block() as block:
            @block.tensor
            def _(e):
                for f in self.q["pe"]:
                    f(e)

            @block.scalar
            def _(e):
                for f in self.q["act"]:
                    f(e)

            @block.vector
            def _(e):
                for f in self.q["dve"]:
                    f(e)

            @block.gpsimd
            def _(e):
                for f in self.q["pool"]:
                    f(e)

            @block.sync
            def _(e):
                for f in self.q["sp"]:
                    f(e)

    def mm(self, out, lhsT, rhs, start, stop, r, w):
        return self.op("pe", lambda e: e.matmul(out, lhsT, rhs, start=start, stop=stop), r, w)

    def tr(self, out, in_, ident, r, w):
        return self.op("pe", lambda e: e.transpose(out, in_, ident), r, w)

    def act(self, eng, out, in_, func, r, w, bias=None, scale=1.0, accum=None):
        def fn(e):
            kw = {}
            if bias is not None:
                kw["bias"] = bias
            if accum is not None:
                kw["accum_out"] = accum
            return e.activation(out, in_, func, scale=scale, **kw)
        return self.op(eng, fn, r, w)

    def ts(self, eng, out, in0, s1, s2, op0, op1, r, w):
        if op1 is None:
            return self.op(eng, lambda e: e.tensor_scalar(out, in0, s1, None, op0), r, w)
        return self.op(eng, lambda e: e.tensor_scalar(out, in0, s1, s2, op0, op1), r, w)

    def tt(self, eng, out, in0, in1, op, r, w):
        return self.op(eng, lambda e: e.tensor_tensor(out, in0, in1, op), r, w)

    def stt(self, out, in0, scalar, in1, op0, op1, r, w):
        return self.op("dve", lambda e: e.scalar_tensor_tensor(out, in0, scalar, in1, op0, op1), r, w)

    def copy(self, eng, out, in_, r, w):
        if eng == "act":
            return self.op("act", lambda e: e.copy(out, in_), r, w)
        return self.op(eng, lambda e: e.tensor_copy(out, in_), r, w)

    def ld(self, out, in_, semkey, r, w, queue="sp"):
        return self.dma(queue, lambda e: e.dma_start(out, in_), semkey, r, w)

    def st(self, out, in_, semkey, r, w, queue="pool", is_out=False):
        return self.dma(queue, lambda e: e.dma_start(out, in_), semkey, r, w, is_out=is_out)


_UNIQ = [0]
DECL_INPUTS = []


def uniq(name):
    _UNIQ[0] += 1
    return f"sb{_UNIQ[0]}_{name}"


def bcast(ap, shape):
    return ap.to_broadcast(shape)


def build(stage="full", dbg=False):
    nc = bass.Bass("TRN2", target_bir_lowering=False)
    es = ExitStack()
    with es:
        _build(nc, es, stage, dbg)
    return nc


def _build(nc, es, stage, dbg):
    P = Prog(nc, es)

    DECL_INPUTS.clear()

    def dram_in(name, shape, dt=F32):
        DECL_INPUTS.append(name)
        return nc.dram_tensor(name, list(shape), dt, kind="ExternalInput").ap()

    def dram_scr(name, shape, dt=BF16):
        kind = "ExternalOutput" if (dbg and name in DBG_OUT) else "Internal"
        return nc.dram_tensor(name, list(shape), dt, kind=kind).ap()

    def sb(name, shape, dt=F32):
        return es.enter_context(nc.sbuf_tensor(uniq(name), list(shape), dt))

    x_ext = dram_in("x_ext", [TEXT, D])
    w_in = dram_in("w_in", [D, DIN])
    g_mix_t = dram_in("g_mix_t", [128, 8])
    ident_d = dram_in("ident", [128, 128])
    out_d = nc.dram_tensor("out", [TOWN, D], F32, kind="ExternalOutput").ap()

    QGT = dram_scr("QGT", [64, 4, TOWN])
    KGT = dram_scr("KGT", [64, 4, TEXT])
    AT = dram_scr("AT", [16, TEXT])
    QMT = dram_scr("QMT", [8, 64, TOWN])
    KMT = dram_scr("KMT", [8, 64, TEXT])
    ZGT = dram_scr("ZGT", [8, 128, TOWN])
    ZMT = dram_scr("ZMT", [8, 128, TOWN])
    VG = dram_scr("VG", [TEXT, 512])
    KG = dram_scr("KG", [TEXT, 256])
    RG = dram_scr("RG", [TOWN, 512])
    VM = dram_scr("VM", [TEXT, 512])

    ps_all = es.enter_context(nc.psum_tensor("ps_all", [128, 4096], F32))
    ps = [ps_all[:, i * 512:(i + 1) * 512] for i in range(8)]
    PSK = [f"ps{i}" for i in range(8)]

    ident_f = sb("ident_f", [128, 128])
    ident_b = sb("ident_b", [128, 128], BF16)
    P.ld(ident_f[:], ident_d, "c0", [], ["ident_f"])
    P.copy("dve", ident_b[:], ident_f[:], ["ident_f"], ["ident_b"])
    gmix = sb("gmix", [128, 8])
    P.ld(gmix[:], g_mix_t, "c1", [], ["gmix"])

    with ExitStack() as pes:
        def psb(name, shape, dt=F32):
            return pes.enter_context(nc.sbuf_tensor(uniq(name), list(shape), dt))
        win_b = psb("win_b", [128, 8, DIN], BF16)
        w_in_v = w_in.rearrange("(k p) n -> p k n", p=128)
        wblocks = [(C_KG, 256), (C_A, 16), (C_KM, 512), (C_VG, 512), (C_VM, 512),
                   (C_QG, 256), (C_QM, 512), (C_ZG, 512), (C_ZG + 512, 512), (C_ZM, 512), (C_ZM + 512, 512), (C_RG, 512)]
        wkeys = {}
        for bi, (c0_, n_) in enumerate(wblocks):
            for c_ in range(c0_, c0_ + n_, 16):
                wkeys[c_] = f"win{bi}"
            P.dma("pool", lambda e, c0_=c0_, n_=n_: e.dma_start(win_b[:, :, c0_:c0_ + n_], w_in_v[:, :, c0_:c0_ + n_]),
                  f"winq{bi}", [], [f"win{bi}"])

        def wkey(c0_, n_):
            return sorted({wkeys[c_] for c_ in range(c0_, c0_ + n_, 16)})

        xt = [psb(f"xt{i}", [128, D]) for i in range(4)]
        junk = psb("junk", [128, D], BF16)
        xn = [psb(f"xn{i}", [128, D], BF16) for i in range(4)]
        stat = [psb(f"stat{i}", [128, 4]) for i in range(4)]
        hT = [psb(f"hT{i}", [128, 8, 512], BF16) for i in range(2)]
        s_qg = psb("s_qg", [128, 2, 512], BF16)
        s_kg = psb("s_kg", [128, 2, 512], BF16)
        s_a = psb("s_a", [16, 512], BF16)
        s_qm = psb("s_qm", [128, 4, 512], BF16)
        s_km = psb("s_km", [128, 4, 512], BF16)
        s_zg = psb("s_zg", [128, 8, 512], BF16)
        s_zm = psb("s_zm", [128, 8, 512], BF16)
        s_vg = psb("s_vg", [128, 4, 512], BF16)
        s_kgt = psb("s_kgt", [128, 4, 256], BF16)
        s_rg = psb("s_rg", [128, 4, 512], BF16)
        s_vm = psb("s_vm", [128, 4, 512], BF16)

        x_v = x_ext.rearrange("(n p) d -> n p d", p=128)
        ti = 0
        pbank = [0]

        def nextbank():
            b = pbank[0]
            pbank[0] = (b + 1) % 6
            return b

        evq = [0]

        def evac(out, in_, r, w, scale=None):
            e = ["act", "dve"][evq[0] % 2]
            evq[0] += 1
            if scale is None:
                P.copy(e, out, in_, r, w)
            elif e == "act":
                P.act("act", out, in_, AF.Copy, r, w, scale=scale)
            else:
                P.ts("dve", out, in_, scale, None, ALU.mult, None, r, w)

        NG = TEXT // 512

        def ab_stats(g):
            for t in range(4):
                i = g * 4 + t
                P.ld(xt[t][:], x_v[i], f"xt{t}", [], [f"xt{t}"])
                P.act("act", junk[:], xt[t][:], AF.Square, [f"xt{t}"], ["junk", f"stat{t}"], accum=stat[t][:, 0:1])
                P.ts("dve", stat[t][:, 1:2], stat[t][:, 0:1], 1.0 / D, EPS, ALU.mult, ALU.add, [f"stat{t}"], [f"stat{t}b"])
                P.act("act", stat[t][:, 2:3], stat[t][:, 1:2], AF.Ln, [f"stat{t}b"], [f"stat{t}c"])
                P.act("act", stat[t][:, 3:4], stat[t][:, 2:3], AF.Exp, [f"stat{t}c"], [f"stat{t}d"], scale=-0.5)
                P.ts("dve", xn[t][:], xt[t][:], stat[t][:, 3:4], None, ALU.mult, None, [f"xt{t}", f"stat{t}d"], [f"xn{t}"])

        def ab_tr(g):
            hs = g % 2
            hk = f"hT{hs}"
            for t in range(4):
                tb = 6 + (t % 2)
                pt = ps[tb][:].bitcast(BF16)
                for k in range(8):
                    P.tr(pt[:, k * 128:(k + 1) * 128], xn[t][:, k * 128:(k + 1) * 128], ident_b[:],
                         [f"xn{t}", "ident_b"], [PSK[tb]])
                P.tt("dve", hT[hs][:, :, t * 128:(t + 1) * 128], pt.rearrange("p (k t) -> p k t", k=8),
                     bcast(gmix[:].unsqueeze(2), [128, 8, 128]), ALU.mult, [PSK[tb], "gmix"], [hk])

        def ab_parts(g):
            own = g >= NG // 2
            go = g - NG // 2
            hs = g % 2
            hk = f"hT{hs}"
            parts = []

            def fm(c0, m, dst, dkey, scale=None):
                def f_():
                    b = nextbank()
                    for k in range(8):
                        P.mm(ps[b][0:m, :], win_b[:, k, c0:c0 + m], hT[hs][:, k, :], k == 0, k == 7,
                             wkey(c0, m) + [hk], [PSK[b]])
                    evac(dst, ps[b][0:m, :], [PSK[b]], [dkey], scale)
                parts.append(f_)

            def tm(c0, n, dst3, dkey):
                for t in range(4):
                    def f_(t=t):
                        b = nextbank()
                        for k in range(8):
                            P.mm(ps[b][:, 0:n], hT[hs][:, k, t * 128:(t + 1) * 128], win_b[:, k, c0:c0 + n], k == 0, k == 7,
                                 wkey(c0, n) + [hk], [PSK[b]])
                        evac(dst3[:, t, :], ps[b][:, 0:n], [PSK[b]], [dkey])
                    parts.append(f_)

            def st_(fn):
                parts.append(fn)

            tsl = slice(g * 512, (g + 1) * 512)
            osl = slice(go * 512, (go + 1) * 512)
            for pr in range(2):
                fm(C_KG + pr * 128, 128, s_kg[:, pr, :], "s_kg")
            st_(lambda: [P.st(KGT.rearrange("d (pr hh) t -> d hh pr t", hh=2)[:, hh, :, tsl], s_kg[hh * 64:(hh + 1) * 64, :, :], "s_kg", ["s_kg"], ["KGT"]) for hh in range(2)])
            fm(C_A, 16, s_a[:], "s_a")
            st_(lambda: P.st(AT[:, tsl], s_a[:], "s_a", ["s_a"], ["AT"]))
            for pr in range(4):
                fm(C_KM + pr * 128, 128, s_km[:, pr, :], "s_km")
            st_(lambda: [P.st(KMT.rearrange("(pr hh) d t -> hh d pr t", hh=2)[hh][:, :, tsl], s_km[hh * 64:(hh + 1) * 64, :, :], "s_km", ["s_km"], ["KMT"]) for hh in range(2)])
            tm(C_VG, 512, s_vg, "s_vg")
            st_(lambda: P.st(VG[tsl, :].rearrange("(t p) f -> p t f", p=128), s_vg[:], "s_vg", ["s_vg"], ["VG"]))
            tm(C_KG, 256, s_kgt, "s_kgt")
            st_(lambda: P.st(KG[tsl, :].rearrange("(t p) f -> p t f", p=128), s_kgt[:], "s_kgt", ["s_kgt"], ["KG"]))
            tm(C_VM, 512, s_vm, "s_vm")
            st_(lambda: P.st(VM[tsl, :].rearrange("(t p) f -> p t f", p=128), s_vm[:], "s_vm", ["s_vm"], ["VM"]))
            if own:
                for pr in range(2):
                    fm(C_QG + pr * 128, 128, s_qg[:, pr, :], "s_qg")
                st_(lambda: [P.st(QGT.rearrange("d (pr hh) t -> d hh pr t", hh=2)[:, hh, :, osl], s_qg[hh * 64:(hh + 1) * 64, :, :], "s_qg", ["s_qg"], ["QGT"]) for hh in range(2)])
                for pr in range(4):
                    fm(C_QM + pr * 128, 128, s_qm[:, pr, :], "s_qm", scale=0.125)
                st_(lambda: [P.st(QMT.rearrange("(pr hh) d t -> hh d pr t", hh=2)[hh][:, :, osl], s_qm[hh * 64:(hh + 1) * 64, :, :], "s_qm", ["s_qm"], ["QMT"]) for hh in range(2)])
                for c in range(8):
                    fm(C_ZG + c * 128, 128, s_zg[:, c, :], "s_zg")
                st_(lambda: P.st(ZGT[:, :, osl].rearrange("c p t -> p c t"), s_zg[:], "s_zg", ["s_zg"], ["ZGT"]))
                for c in range(8):
                    fm(C_ZM + c * 128, 128, s_zm[:, c, :], "s_zm")
                st_(lambda: P.st(ZMT[:, :, osl].rearrange("c p t -> p c t"), s_zm[:], "s_zm", ["s_zm"], ["ZMT"]))
                tm(C_RG, 512, s_rg, "s_rg")
                st_(lambda: P.st(RG[osl, :].rearrange("(t p) f -> p t f", p=128), s_rg[:], "s_rg", ["s_rg"], ["RG"]))
            return parts

        ab_stats(0)
        ab_tr(0)
        for g in range(NG):
            parts = ab_parts(g)
            if g + 1 < NG:
                ab_stats(g + 1)
            half = len(parts) // 2
            for f_ in parts[:half]:
                f_()
            if g + 1 < NG:
                ab_tr(g + 1)
            for f_ in parts[half:]:
                f_()
        P.barrier()

    def stop_here():
        z = sb("zout_" + stage, [128, D])
        P.op("dve", lambda e: e.memset(z[:], 0.0), [], ["zout"])
        P.st(out_d[0:128, :], z[:], "zo", ["zout"], ["out"], is_out=True)
        P.finish()

    if stage == "AB":
        stop_here()
        return

    def load_cast(dst, src, n, stg_tiles, ci, dkey, parts=128, engs=("pool", "dve", "act")):
        s = ci[0] % len(stg_tiles)
        shp = list(dst.shape)
        F = 1
        for d_ in shp[1:]:
            F *= d_
        sv = stg_tiles[s][0:parts, 0:F]
        if len(shp) == 3:
            sv = sv.rearrange("p (a b) -> p a b", a=shp[1])
        P.ld(sv, src, f"wstg{s}", [], [f"wstg{s}"])
        P.copy(engs[ci[0] % len(engs)], dst, sv, [f"wstg{s}"], [dkey])
        ci[0] += 1

    XS = dram_scr("XS", [NROWS, D], BF16)
    zt = sb("zt", [128, D], BF16)
    P.op("pool", lambda e: e.memset(zt[:], 0.0), [], ["zt"])
    XSv = XS.rearrange("(t p) d -> t p d", p=128)
    zfill_i = [0]

    def zfill(n):
        for _ in range(n):
            if zfill_i[0] < NROWS // 128:
                P.st(XSv[zfill_i[0]], zt[:], "zfill", ["zt"], ["XS"], queue="sp")
                zfill_i[0] += 1

    w_up_d = dram_in("w_alpha_up", [16, 256])
    b_al_d = dram_in("b_alpha", [1, 256])
    u2_d = dram_in("u2", [128, 128])
    u3_d = dram_in("u3", [128, 128])
    maskT_d = dram_in("maskT", [64, 64])
    ggla_d = dram_in("ggla_rep", [64, 512])
    OGT = dram_scr("OGT", [4, 128, TOWN])
    with ExitStack() as pes:
        def psb(name, shape, dt=F32):
            return pes.enter_context(nc.sbuf_tensor(uniq(name), list(shape), dt))
        wup_f = psb("wup_f", [16, 256]); wup_b = psb("wup_b", [16, 256], BF16)
        bal_f = psb("bal_f", [1, 256]); bal_b = psb("bal_b", [1, 256], BF16)
        ones_row = psb("ones_row", [1, 128], BF16)
        U2 = psb("U2", [128, 128]); maskT = psb("maskT", [64, 64]); ggla = psb("ggla", [64, 512])
        U3 = psb("U3", [128, 128])
        P.ld(U3[:], u3_d, "k5", [], ["U3"])
        P.ld(wup_f[:], w_up_d, "k0", [], ["wup_f"]); P.copy("dve", wup_b[:], wup_f[:], ["wup_f"], ["wup_b"])
        P.ld(bal_f[:], b_al_d, "k1", [], ["bal_f"]); P.copy("dve", bal_b[:], bal_f[:], ["bal_f"], ["bal_b"])
        P.op("dve", lambda e: e.memset(ones_row[:], 1.0), [], ["ones_row"])
        P.ld(U2[:], u2_d, "k2", [], ["U2"]); P.ld(maskT[:], maskT_d, "k3", [], ["maskT"]); P.ld(ggla[:], ggla_d, "k4", [], ["ggla"])
        state_f = psb("state_f", [64, 512]); state_b = psb("state_b", [64, 4, 128], BF16)
        P.op("dve", lambda e: e.memset(state_f[:], 0.0), [], ["state_f"])
        P.op("dve", lambda e: e.memset(state_b[:], 0.0), [], ["state_b"])
        kTg = [psb(f"kTg{i}", [64, 4, 512], BF16) for i in range(2)]
        qTg = [psb(f"qTg{i}", [64, 4, 512], BF16) for i in range(2)]
        aTg = [psb(f"aTg{i}", [16, 512], BF16) for i in range(2)]
        vg = [psb(f"vg{i}", [64, 8, 512], BF16) for i in range(2)]
        kg = [psb(f"kg{i}", [64, 8, 256], BF16) for i in range(2)]
        rg = [psb(f"rg{i}", [64, 8, 512], BF16) for i in range(2)]
        ogTg = [psb(f"ogTg{i}", [128, 4, 512], BF16) for i in range(2)]
        e1 = psb("e1", [128, 256]); nla = psb("nla", [128, 256])
        EBt = [psb(f"EBt{i}", [64, 512]) for i in range(3)]
        EpT = [psb(f"EpT{i}", [64, 512]) for i in range(3)]
        EmT = [psb(f"EmT{i}", [64, 512]) for i in range(3)]
        Kpt = [psb(f"Kpt{i}", [64, 2, 256], BF16) for i in range(3)]
        KpT = [psb(f"KpT{i}", [64, 4, 128], BF16) for i in range(3)]
        QpT = [psb(f"QpT{i}", [64, 4, 128], BF16) for i in range(3)]
        attT = psb("attT", [64, 4, 64], BF16)
        o_sb2 = [psb(f"o_sb{i}", [64, 2, 512]) for i in range(2)]; tmp = psb("tmp", [64, 512])
        sq = psb("sq", [64, 1024]); st8 = psb("st8", [64, 32])
        og1 = psb("og1", [64, 1024]); og2 = psb("og2", [64, 1024]); sr = psb("sr", [64, 1024]); og = psb("og", [64, 2, 512], BF16)
        NG = TEXT // 512
        tiles = [(g, t) for g in range(NG) for t in range(4)]

        def c_loads(g):
            own = g >= NG // 2
            go = g - NG // 2
            s = g % 2
            tsl = slice(g * 512, (g + 1) * 512)
            osl = slice(go * 512, (go + 1) * 512)
            P.ld(kTg[s][:], KGT[:, :, tsl], f"kTg{s}", ["KGT"], [f"kTg{s}"])
            P.ld(aTg[s][:], AT[:, tsl], f"aTg{s}", ["AT"], [f"aTg{s}"])
            P.ld(vg[s][:], VG[tsl, :].rearrange("(c p) f -> p c f", p=64), f"vg{s}", ["VG"], [f"vg{s}"])
            P.ld(kg[s][:], KG[tsl, :].rearrange("(c p) f -> p c f", p=64), f"kg{s}", ["KG"], [f"kg{s}"])
            if own:
                P.ld(qTg[s][:], QGT[:, :, osl], f"qTg{s}", ["QGT"], [f"qTg{s}"])
                P.ld(rg[s][:], RG[osl, :].rearrange("(c p) f -> p c f", p=64), f"rg{s}", ["RG"], [f"rg{s}"])

        def ph1(idx):
            g, t = tiles[idx]
            own = g >= NG // 2
            s = g % 2
            p = idx % 3
            tc = slice(t * 128, (t + 1) * 128)
            P.mm(ps[0][:, 0:256], aTg[s][:, tc], wup_b[:], True, False, [f"aTg{s}", "wup_b"], ["ps0"])
            P.mm(ps[0][:, 0:256], ones_row[:], bal_b[:], False, True, ["ones_row", "bal_b"], ["ps0"])
            P.act("act", e1[:], ps[0][:, 0:256], AF.Exp, ["ps0"], ["e1"], scale=-1.0)
            P.act("act", nla[:], e1[:], AF.Ln, ["e1"], ["nla"], bias=1.0)
            for j in range(2):
                P.mm(ps[1][0:64, j * 256:(j + 1) * 256], U3[:, j * 64:(j + 1) * 64], nla[:], True, True, ["U3", "nla"], ["ps1"])
            for h in range(4):
                P.mm(ps[2][0:64, h * 128:(h + 1) * 128], nla[:, h * 64:(h + 1) * 64], U2[:], True, True, ["U2", "nla"], ["ps2"])
            P.act("act", EBt[p][:], ps[1][0:64, :], AF.Exp, ["ps1"], [f"EBt{p}"], scale=-1.0 / 16)
            P.tt("dve", Kpt[p][:], kg[s][:, 2 * t:2 * t + 2, :], EBt[p][:].rearrange("p (j f) -> p j f", j=2), ALU.mult,
                 [f"kg{s}", f"EBt{p}"], [f"Kpt{p}"])
            P.act("act", EmT[p][:], ps[2][0:64, :], AF.Exp, ["ps2"], [f"EmT{p}"], scale=-1.0 / 16)
            if own:
                P.act("act", EpT[p][:], ps[2][0:64, :], AF.Exp, ["ps2"], [f"EpT{p}"], scale=1.0 / 16)
                P.tt("pool", KpT[p][:], kTg[s][:, :, tc], EpT[p][:].rearrange("p (h c) -> p h c", h=4), ALU.mult,
                     [f"kTg{s}", f"EpT{p}"], [f"KpT{p}"])
                P.stt(QpT[p][:], qTg[s][:, :, tc], 0.125, EmT[p][:].rearrange("p (h c) -> p h c", h=4), ALU.mult, ALU.mult,
                      [f"qTg{s}", f"EmT{p}"], [f"QpT{p}"])

        def ph2(idx):
            g, t = tiles[idx]
            own = g >= NG // 2
            go = g - NG // 2
            s = g % 2
            p = idx % 3
            tc = slice(t * 128, (t + 1) * 128)
            EmT3 = EmT[p][:].rearrange("p (h c) -> p h c", h=4)
            o_sb = o_sb2[idx % 2]
            ok_ = f"o_sb{idx % 2}"
            for j in range(2):
                cc = slice(j * 64, (j + 1) * 64)
                ch = 2 * t + j
                if own:
                    for h in range(4):
                        P.mm(ps[3][0:64, h * 64:(h + 1) * 64], KpT[p][:, h, cc], QpT[p][:, h, cc], True, True, [f"KpT{p}", f"QpT{p}"], ["ps3"])
                    P.tt("dve", attT[:], ps[3][0:64, 0:256].rearrange("p (h c) -> p h c", h=4),
                         bcast(maskT[:].unsqueeze(1), [64, 4, 64]), ALU.mult, ["ps3", "maskT"], ["attT"])
                    for h in range(4):
                        P.mm(ps[4][0:64, h * 128:(h + 1) * 128], attT[:, h, :], vg[s][:, ch, h * 128:(h + 1) * 128], True, False,
                             ["attT", f"vg{s}"], ["ps4"])
                        P.mm(ps[4][0:64, h * 128:(h + 1) * 128], QpT[p][:, h, cc], state_b[:, h, :], False, True,
                             [f"QpT{p}", "state_b"], ["ps4"])
                    P.copy("act", o_sb[:, j, :], ps[4][0:64, :], ["ps4"], [ok_])
                for h in range(4):
                    P.mm(ps[5][0:64, h * 128:(h + 1) * 128], Kpt[p][:, j, h * 64:(h + 1) * 64], vg[s][:, ch, h * 128:(h + 1) * 128], True, True,
                         [f"Kpt{p}", f"vg{s}"], ["ps5"])
                dec = bcast(EmT3[:, :, j * 64 + 63:j * 64 + 64], [64, 4, 128])
                tmp3 = tmp[:].rearrange("p (h c) -> p h c", h=4)
                P.tt("dve", tmp3, state_f[:].rearrange("p (h c) -> p h c", h=4), dec, ALU.mult, ["state_f", f"EmT{p}"], ["tmp"])
                P.tt("dve", state_b[:].rearrange("p h c -> p (h c)"), tmp[:], ps[5][0:64, :], ALU.add, ["tmp", "ps5"], ["state_b"])
                P.tt("dve", state_f[:], tmp[:], ps[5][0:64, :], ALU.add, ["tmp", "ps5"], ["state_f"])

        def ph3(idx):
            g, t = tiles[idx]
            own = g >= NG // 2
            go = g - NG // 2
            s = g % 2
            tc = slice(t * 128, (t + 1) * 128)
            o_sb = o_sb2[idx % 2]
            ok_ = f"o_sb{idx % 2}"
            if own:
                of = o_sb[:].rearrange("p j f -> p (j f)")
                P.tt("pool", sq[:], of, of, ALU.mult, [ok_], ["sq"])
                P.op("dve", lambda e: e.tensor_reduce(st8[:, 0:8], sq[:].rearrange("p (a b) -> p a b", a=8), AX.X, ALU.add),
                     ["sq"], ["st8a"])
                P.ts("dve", st8[:, 8:16], st8[:, 0:8], 1.0 / 128, EPS, ALU.mult, ALU.add, ["st8a"], ["st8b"])
                P.act("act", st8[:, 16:24], st8[:, 8:16], AF.Ln, ["st8b"], ["st8c"])
                P.act("act", st8[:, 24:32], st8[:, 16:24], AF.Exp, ["st8c"], ["st8d"], scale=-0.5)
                P.tt("dve", og1[:].rearrange("p (a b) -> p a b", a=8), of.rearrange("p (a b) -> p a b", a=8),
                     bcast(st8[:, 24:32].unsqueeze(2), [64, 8, 128]), ALU.mult, [ok_, "st8d"], ["og1"])
                P.tt("pool", og2[:].rearrange("p (j f) -> p j f", j=2), og1[:].rearrange("p (j f) -> p j f", j=2),
                     bcast(ggla[:].unsqueeze(1), [64, 2, 512]), ALU.mult, ["og1", "ggla"], ["og2"])
                P.act("act", sr[:].rearrange("p (j f) -> p j f", j=2), rg[s][:, 2 * t:2 * t + 2, :], AF.Silu, [f"rg{s}"], ["sr"])
                P.tt("dve", og[:].rearrange("p j f -> p (j f)"), og2[:], sr[:], ALU.mult, ["og2", "sr"], ["og"])
                pt = ps[6][:].bitcast(BF16)
                for kc in range(4):
                    for j in range(2):
                        P.tr(pt[:, kc * 128 + j * 64:kc * 128 + (j + 1) * 64], og[:, j, kc * 128:(kc + 1) * 128], ident_b[0:64, 0:64],
                             ["og", "ident_b"], ["ps6"])
                P.copy("act", ogTg[s][:, :, tc], pt[:, 0:512].rearrange("p (k c) -> p k c", k=4), ["ps6"], [f"ogTg{s}"])
                if t == 3:
                    osl = slice(go * 512, (go + 1) * 512)
                    P.st(OGT[:, :, osl].rearrange("c p t -> p c t"), ogTg[s][:], f"ogTg{s}", [f"ogTg{s}"], ["OGT"])

        c_loads(0)
        zfill(6)
        ph1(0)
        ph1(1)
        for idx in range(len(tiles)):
            if idx + 2 < len(tiles):
                g2, t2 = tiles[idx + 2]
                if t2 == 0:
                    c_loads(g2)
                    zfill(6)
                ph1(idx + 2)
            ph2(idx)
            if idx >= 1:
                ph3(idx - 1)
        ph3(len(tiles) - 1)
        P.barrier()
    if stage == "C":
        stop_here()
        return

    wpg_d = dram_in("w_proj_gla", [512, D]); wpm_d = dram_in("w_proj_moba", [512, D]); wout_d = dram_in("w_out", [D, D])
    wpg_b = sb("wpg_b", [128, 4, D], BF16); wpm_b = sb("wpm_b", [128, 4, D], BF16); wout_b = sb("wout_b", [128, 8, D], BF16)
    P.dma("pool", lambda e: e.dma_start(wpg_b[:], wpg_d.rearrange("(k p) n -> p k n", p=128)), "pfE0", [], ["wpg_b"])
    P.dma("pool", lambda e: e.dma_start(wpm_b[:], wpm_d.rearrange("(k p) n -> p k n", p=128)), "pfE1", [], ["wpm_b"])
    for hh_ in range(2):
        P.dma("pool", lambda e, hh_=hh_: e.dma_start(wout_b[:, hh_ * 4:(hh_ + 1) * 4, :],
              wout_d[hh_ * 512:(hh_ + 1) * 512, :].rearrange("(k p) n -> p k n", p=128)), "pfE2", [], ["wout_b"])
    ind_d = dram_in("ind", [32, TEXT], BF16)
    tb_d = dram_in("tb", [8, 128, 6, 512], BF16)
    gadd_d = dram_in("gadd", [128, 32, 32])
    pv01_d = dram_in("pv01", [128, 32, 32])
    bfix_d = dram_in("bfix", [128, 32, 32])
    cfm_d = dram_in("cfm", [128, 32, 32])
    cfar_d = dram_in("cfar_rep", [128, 8])
    sh_d = dram_in("sh", [128, 64])
    OMT = dram_scr("OMT", [4, 128, TOWN])
    with ExitStack() as pes:
        def psb(name, shape, dt=F32):
            return pes.enter_context(nc.sbuf_tensor(uniq(name), list(shape), dt))
        kTa = [psb(f"kTa{i}", [96, TEXT], BF16) for i in range(2)]
        Va = [psb(f"Va{i}", [128, 64, 128], BF16) for i in range(2)]
        qTa = [psb(f"qTa{i}", [96, TOWN], BF16) for i in range(2)]
        Tb = [psb(f"Tb{i}", [128, 6, 512], BF16) for i in range(2)]
        gadd = psb("gadd", [128, 32, 32]); pv01 = psb("pv01", [128, 32, 32]); bfix = psb("bfix", [128, 32, 32]); cfm = psb("cfm", [128, 32, 32])
        cfar = psb("cfar", [128, 8]); Sh = psb("Sh", [128, 64])
        P.ld(gadd[:], gadd_d, "m0", [], ["gadd"]); P.ld(pv01[:], pv01_d, "m1", [], ["pv01"])
        P.ld(bfix[:], bfix_d, "m2", [], ["bfix"]); P.ld(cfm[:], cfm_d, "m3", [], ["cfm"])
        P.ld(cfar[:], cfar_d, "m4", [], ["cfar"]); P.ld(Sh[:], sh_d, "m5", [], ["Sh"])
        for i in range(2):
            P.ld(kTa[i][64:96, :], ind_d, f"ind{i}", [], [f"kTa{i}i"])
            P.op("pool", lambda e, i=i: e.memset(Va[i][:, :, 64:128], 1.0), [], [f"Va{i}o"])
        kmf = psb("kmf", [64, 32]); kmb = psb("kmb", [64, 32], BF16)
        gs = psb("gs", [128, 512]); g2 = psb("g2", [128, 512]); eqt = psb("eqt", [128, 512]); mx = psb("mx", [128, 48]); acoef = psb("acoef", [128, 1024])
        MBw2 = [psb(f"MBw{i}", [128, 16, 96], BF16) for i in range(2)]
        for i in range(2):
            P.op("dve", lambda e, i=i: e.memset(MBw2[i][:], 0.0), [], [f"MBw{i}"])
        PT = [psb(f"PT{i}", [128, 1024], BF16) for i in range(3)]
        obs = [psb(f"obs{i}", [128, 512]) for i in range(2)]
        rec2 = [psb(f"rec{i}", [128, 512]) for i in range(2)]
        pending_tail = []
        omT = [psb(f"omT{i}", [64, 512], BF16) for i in range(2)]
        for i in range(2):
            P.op("dve", lambda e, i=i: e.memset(rec2[i][:], 0.0), [], [f"rec{i}"])
        pti = 0
        sbk = 0

        def d_loads(h):
            s = h % 2
            P.ld(kTa[s][0:64, :], KMT[h], f"kTa{s}", ["KMT"], [f"kTa{s}"])
            for qq in range(4):
                P.ld(Va[s][:, qq * 16:(qq + 1) * 16, 0:64],
                     VM[qq * 2048:(qq + 1) * 2048, h * 64:(h + 1) * 64].rearrange("(t p) f -> p t f", p=128),
                     f"Va{s}", ["VM"], [f"Va{s}"])
            P.ld(qTa[s][0:64, :], QMT[h], f"qTa{s}", ["QMT"], [f"qTa{s}"])
            P.ld(Tb[s][:], tb_d[h], f"Tb{s}", [], [f"Tb{s}"])

        def d_kmean(h):
            s = h % 2
            P.op("dve", lambda e, s=s: e.tensor_reduce(kmf[:], kTa[s][0:64, :].rearrange("p (n k) -> p n k", n=32), AX.X, ALU.add),
                 [f"kTa{s}"], ["kmf"])
            P.copy("dve", kmb[:], kmf[:], ["kmf"], ["kmb"])
            P.ts("pool", acoef[:], cfm[:].rearrange("p a b -> p (a b)"), cfar[:, h:h + 1], -NEG, ALU.mult, ALU.add, ["cfm", "cfar"], ["acoef"])

        def gate_p1(h, hb):
            s = h % 2
            q0 = hb * 16
            for qi in range(16):
                qc = slice((q0 + qi) * 128, (q0 + qi + 1) * 128)
                P.mm(ps[7][:, qi * 32:(qi + 1) * 32], qTa[s][0:64, qc], kmb[:], True, True, [f"qTa{s}", "kmb"], ["ps7"])
            gs3 = gs[:].rearrange("p (a b) -> p a b", a=16)
            g23 = g2[:].rearrange("p (a b) -> p a b", a=16)
            eq3 = eqt[:].rearrange("p (a b) -> p a b", a=16)
            P.tt("dve", gs3, ps[7][:, :].rearrange("p (a b) -> p a b", a=16), gadd[:, q0:q0 + 16, :], ALU.add, ["ps7", "gadd"], ["gs"])
            P.op("dve", lambda e, gs3=gs3: e.tensor_reduce(mx[:, 0:16], gs3, AX.X, ALU.max), ["gs"], ["mx0"])
            P.tt("dve", eq3, gs3, bcast(mx[:, 0:16].unsqueeze(2), [128, 16, 32]), ALU.is_ge, ["gs", "mx0"], ["eqt"])
            P.stt(g2[:], eqt[:], -1e30, gs[:], ALU.mult, ALU.add, ["eqt", "gs"], ["g2"])
            P.op("dve", lambda e, g23=g23: e.tensor_reduce(mx[:, 16:32], g23, AX.X, ALU.max), ["g2"], ["mx1"])
            P.tt("dve", eq3, g23, bcast(mx[:, 16:32].unsqueeze(2), [128, 16, 32]), ALU.is_ge, ["g2", "mx1"], ["eqt"])
            P.stt(g2[:], eqt[:], -1e30, g2[:], ALU.mult, ALU.add, ["eqt", "g2"], ["g2"])
            P.op("dve", lambda e, g23=g23: e.tensor_reduce(mx[:, 32:48], g23, AX.X, ALU.max), ["g2"], ["mx2"])
            P.tt("dve", eq3, gs3, bcast(mx[:, 32:48].unsqueeze(2), [128, 16, 32]), ALU.is_ge, ["gs", "mx2"], ["eqt"])
            P.tt("dve", eq3, eq3, pv01[:, q0:q0 + 16, :], ALU.mult, ["eqt", "pv01"], ["eqt"])
            P.tt("dve", eq3, eq3, acoef[:].rearrange("p (a b) -> p a b", a=32)[:, q0:q0 + 16, :], ALU.mult, ["eqt", "acoef"], ["eqt"])
            P.tt("dve", MBw2[hb][:, :, 64:96], eq3, bfix[:, q0:q0 + 16, :], ALU.add, ["eqt", "bfix"], [f"MBw{hb}"])

        def gate_p2(h, hb):
            s = h % 2
            q0 = hb * 16
            for half in range(2):
                pt = ps[7][:].bitcast(BF16)
                for qi in range(8):
                    P.tr(pt[0:96, qi * 128:(qi + 1) * 128], MBw2[hb][:, half * 8 + qi, :], ident_b[:], [f"MBw{hb}", "ident_b"], ["ps7"])
                c0 = (q0 + half * 8) * 128
                P.copy("act", qTa[s][64:96, c0:c0 + 1024], pt[64:96, :], ["ps7"], [f"qTa{s}m"])

        d_loads(0)
        d_kmean(0)
        gate_p1(0, 0); gate_p2(0, 0); gate_p1(0, 1); gate_p2(0, 1)
        for h in range(8):
            s = h % 2
            if h + 1 < 8:
                d_loads(h + 1)
            for g in range(8):
                osl = slice(g * 512, (g + 1) * 512)
                nkt = 32 + 4 * g + 4
                OB = 6
                npair = nkt // 2

                def emitS(pi):
                    nonlocal sbk
                    db = sbk % 3
                    sbk += 1
                    sdb[pi] = db
                    for u in range(2):
                        kt = 2 * pi + u
                        b = 2 * db + u
                        j = kt - (nkt - 6)
                        P.mm(ps[b][:, :], kTa[s][:, kt * 128:(kt + 1) * 128], qTa[s][:, osl], True, j < 0,
                             [f"kTa{s}", f"kTa{s}i", f"qTa{s}", f"qTa{s}m"], [f"psd{db}"])
                        if j >= 0:
                            P.mm(ps[b][:, :], ident_b[:], Tb[s][:, j, :], False, True, ["ident_b", f"Tb{s}"], [f"psd{db}"])

                sdb = {}
                emitS(0)
                emitS(1)
                for pi in range(npair):
                    if pi + 2 < npair:
                        emitS(pi + 2)
                    if pi == 2:
                        while pending_tail:
                            pending_tail.pop(0)()
                    db = sdb[pi]
                    p = pti % 3
                    pti += 1
                    P.act("act", PT[p][:], ps_all[:, db * 1024:(db + 1) * 1024], AF.Exp, [f"psd{db}"], [f"PT{p}"])
                    for u in range(2):
                        kt = 2 * pi + u
                        P.mm(ps[OB][:, :], Va[s][:, kt, :], PT[p][:, u * 512:(u + 1) * 512], kt == 0, kt == nkt - 1,
                             [f"Va{s}", f"Va{s}o", f"PT{p}"], [PSK[OB]])
                o2 = g % 2
                P.copy("dve", obs[o2][:], ps[OB][:, :], [PSK[OB]], [f"obs{o2}"])
                P.op("dve", lambda e, o2=o2: e.reciprocal(rec2[o2][64:128, :], obs[o2][64:128, :]), [f"obs{o2}"], [f"rec{o2}"])

                def tail(h=h, o2=o2, osl=osl):
                    P.mm(ps[7][0:64, :], Sh[:], rec2[o2][:], True, True, ["Sh", f"rec{o2}"], ["ps7"])
                    P.tt("dve", omT[o2][:], ps[7][0:64, :], obs[o2][0:64, :], ALU.mult, ["ps7", f"obs{o2}"], [f"omT{o2}"])
                    P.st(OMT[h // 2, (h % 2) * 64:(h % 2) * 64 + 64, osl], omT[o2][:], f"omT{o2}", [f"omT{o2}"], ["OMT"])
                pending_tail.append(tail)
                if h + 1 < 8:
                    if g == 0:
                        d_kmean(h + 1)
                        gate_p1(h + 1, 0)
                    elif g == 2:
                        gate_p2(h + 1, 0)
                    elif g == 3:
                        gate_p1(h + 1, 1)
                    elif g == 5:
                        gate_p2(h + 1, 1)
        while pending_tail:
            pending_tail.pop(0)()
        P.barrier()
    if stage == "D":
        stop_here()
        return

    wcq_d = dram_in("w_cq", [D, 512]); wckv_d = dram_in("w_ckv", [D, D]); wco_d = dram_in("w_co", [512, D])
    wcq_b = sb("wcq_b", [128, 8, 512], BF16); wckv_b = sb("wckv_b", [128, 8, D], BF16); wco_b = sb("wco_b", [128, 4, D], BF16)
    P.dma("pool", lambda e: e.dma_start(wcq_b[:], wcq_d.rearrange("(k p) n -> p k n", p=128)), "pfF0", [], ["wcq_b"])
    for hh_ in range(2):
        P.dma("pool", lambda e, hh_=hh_: e.dma_start(wckv_b[:, hh_ * 4:(hh_ + 1) * 4, :],
              wckv_d[hh_ * 512:(hh_ + 1) * 512, :].rearrange("(k p) n -> p k n", p=128)), "pfF1", [], ["wckv_b"])
    P.dma("pool", lambda e: e.dma_start(wco_b[:], wco_d.rearrange("(k p) n -> p k n", p=128)), "pfF2", [], ["wco_b"])
    gcross_d = dram_in("g_cross_t", [128, 8])
    X1 = dram_scr("X1", [TOWN, D], F32)
    H2T = dram_scr("H2T", [8, 128, TOWN])

    def norm_T(xtile, xkey, gt, dstT, dkey, tcs, pes_t, pbank_t):
        junk, stat, xn_ = pes_t
        P.act("act", junk[:], xtile, AF.Square, [xkey], ["junk", "nstat"], accum=stat[:, 0:1])
        P.ts("dve", stat[:, 1:2], stat[:, 0:1], 1.0 / D, EPS, ALU.mult, ALU.add, ["nstat"], ["nstatb"])
        P.act("act", stat[:, 2:3], stat[:, 1:2], AF.Ln, ["nstatb"], ["nstatc"])
        P.act("act", stat[:, 3:4], stat[:, 2:3], AF.Exp, ["nstatc"], ["nstatd"], scale=-0.5)
        P.ts("dve", xn_[:], xtile, stat[:, 3:4], None, ALU.mult, None, [xkey, "nstatd"], ["nxn"])
        pt = ps[pbank_t][:].bitcast(BF16)
        for k in range(8):
            P.tr(pt[:, k * 128:(k + 1) * 128], xn_[:, k * 128:(k + 1) * 128], ident_b[:], ["nxn", "ident_b"], [PSK[pbank_t]])
        P.tt("dve", dstT[:, :, tcs], pt.rearrange("p (k t) -> p k t", k=8), bcast(gt[:].unsqueeze(2), [128, 8, 128]), ALU.mult,
             [PSK[pbank_t], "gT"], [dkey])

    with ExitStack() as pes:
        def psb(name, shape, dt=F32):
            return pes.enter_context(nc.sbuf_tensor(uniq(name), list(shape), dt))
        gT = psb("gT", [128, 8])
        P.ld(gT[:], gcross_d, "e0", [], ["gT"])
        ogT = [psb(f"ogT{i}", [128, 4, 512], BF16) for i in range(2)]
        omTt = [psb(f"omTt{i}", [128, 4, 512], BF16) for i in range(2)]
        zgT = [psb(f"zgT{i}", [128, 8, 512], BF16) for i in range(2)]
        zmT = [psb(f"zmT{i}", [128, 8, 512], BF16) for i in range(2)]
        mgT = psb("mgT", [128, 8, 512], BF16)
        sg = psb("sg", [128, 512]); sm = psb("sm", [128, 512]); m1 = psb("m1", [128, 512]); m2 = psb("m2", [128, 512])
        x1t = [psb(f"x1t{i}", [128, D]) for i in range(4)]
        junk = psb("ejunk", [128, D], BF16); stat4 = psb("estat4", [128, 4, 4]); xn2 = [psb(f"exn{i}", [128, D], BF16) for i in range(2)]
        h2Tg = [psb(f"h2Tg{i}", [128, 8, 512], BF16) for i in range(2)]
        x_v = x_ext.rearrange("(n p) d -> n p d", p=128)
        X1v = X1.rearrange("(n p) d -> n p d", p=128)
        for g in range(8):
            s = g % 2
            osl = slice(g * 512, (g + 1) * 512)
            P.ld(ogT[s][:], OGT[:, :, osl].rearrange("c p t -> p c t"), f"ogT{s}", ["OGT"], [f"ogT{s}"])
            P.ld(omTt[s][:], OMT[:, :, osl].rearrange("c p t -> p c t"), f"omTt{s}", ["OMT"], [f"omTt{s}"])
            P.ld(zgT[s][:], ZGT[:, :, osl].rearrange("c p t -> p c t"), f"zgT{s}", ["ZGT"], [f"zgT{s}"])
            P.ld(zmT[s][:], ZMT[:, :, osl].rearrange("c p t -> p c t"), f"zmT{s}", ["ZMT"], [f"zmT{s}"])
            for n in range(8):
                ba, bb = (2 * n) % 4, (2 * n + 1) % 4
                nsl = slice(n * 128, (n + 1) * 128)
                for k in range(4):
                    P.mm(ps[ba][:, :], wpg_b[:, k, nsl], ogT[s][:, k, :], k == 0, k == 3, ["wpg_b", f"ogT{s}"], [PSK[ba]])
                for k in range(4):
                    P.mm(ps[bb][:, :], wpm_b[:, k, nsl], omTt[s][:, k, :], k == 0, k == 3, ["wpm_b", f"omTt{s}"], [PSK[bb]])
                P.act("act", sg[:], zgT[s][:, n, :], AF.Sigmoid, [f"zgT{s}"], ["sg"])
                P.act("act", sm[:], zmT[s][:, n, :], AF.Sigmoid, [f"zmT{s}"], ["sm"])
                P.tt("dve", m1[:], ps[ba][:, :], sg[:], ALU.mult, [PSK[ba], "sg"], ["m1"])
                P.tt("dve", m2[:], ps[bb][:, :], sm[:], ALU.mult, [PSK[bb], "sm"], ["m2"])
                P.tt("pool", mgT[:, n, :], m1[:], m2[:], ALU.add, ["m1", "m2"], ["mgT"])
            for t in range(4):
                i = g * 4 + t
                tcs = slice(t * 128, (t + 1) * 128)
                P.ld(x1t[t][:], x_v[32 + i], f"x1l{t}", [], [f"x1t{t}"])
                for hf in range(2):
                    b = 4 + hf
                    for k in range(8):
                        P.mm(ps[b][:, :], mgT[:, k, tcs], wout_b[:, k, hf * 512:(hf + 1) * 512], k == 0, k == 7, ["mgT", "wout_b"], [PSK[b]])
                    P.tt("dve", x1t[t][:, hf * 512:(hf + 1) * 512], ps[b][:, :], x1t[t][:, hf * 512:(hf + 1) * 512], ALU.add,
                         [PSK[b], f"x1t{t}"], [f"x1t{t}"])
                P.st(X1v[i], x1t[t][:], f"x1t{t}", [f"x1t{t}"], ["X1"])
                P.act("act", junk[:], x1t[t][:], AF.Square, [f"x1t{t}"], ["junk", f"es{t}a"], accum=stat4[:, t, 0:1])
                P.ts("dve", stat4[:, t, 1:2], stat4[:, t, 0:1], 1.0 / D, EPS, ALU.mult, ALU.add, [f"es{t}a"], [f"es{t}b"])
                P.act("act", stat4[:, t, 2:3], stat4[:, t, 1:2], AF.Ln, [f"es{t}b"], [f"es{t}c"])
                P.act("act", stat4[:, t, 3:4], stat4[:, t, 2:3], AF.Exp, [f"es{t}c"], [f"es{t}d"], scale=-0.5)
            for t in range(4):
                tcs = slice(t * 128, (t + 1) * 128)
                xb = t % 2
                P.ts("dve", xn2[xb][:], x1t[t][:], stat4[:, t, 3:4], None, ALU.mult, None, [f"x1t{t}", f"es{t}d"], [f"exn{xb}"])
                pb = 6 + xb
                pt = ps[pb][:].bitcast(BF16)
                for k in range(8):
                    P.tr(pt[:, k * 128:(k + 1) * 128], xn2[xb][:, k * 128:(k + 1) * 128], ident_b[:], [f"exn{xb}", "ident_b"], [PSK[pb]])
                P.tt(["dve", "pool"][0], h2Tg[s][:, :, tcs], pt.rearrange("p (k t) -> p k t", k=8), bcast(gT[:].unsqueeze(2), [128, 8, 128]), ALU.mult,
                     [PSK[pb], "gT"], [f"h2Tg{s}"])
            P.st(H2T[:, :, osl].rearrange("c p t -> p c t"), h2Tg[s][:], f"h2Tg{s}", [f"h2Tg{s}"], ["H2T"])
        P.barrier()
    if stage == "E":
        stop_here()
        return

    mem_d = dram_in("mem", [256, D]); gmem_d = dram_in("g_mem_t", [128, 8])
    gmoe_d = dram_in("g_moe_rep", [128, D]); wr_d = dram_in("w_router", [D, 36]); br_d = dram_in("b_router_rep", [128, 36])
    slt_d = dram_in("slt", [128, 128]); eoff_d = dram_in("eoff_rep", [128, 32])
    X2 = dram_scr("X2", [TOWN, D], F32)
    destall = sb("destall", [128, 32, 2], I32)
    wall = sb("wall", [128, 32, 2])
    with ExitStack() as pes:
        def psb(name, shape, dt=F32):
            return pes.enter_context(nc.sbuf_tensor(uniq(name), list(shape), dt))
        wr_f = psb("wr_f", [128, 8, 36]); wr_b = psb("wr_b", [128, 8, 36], BF16)
        P.ld(wr_f[:], wr_d.rearrange("(k p) n -> p k n", p=128), "f0", [], ["wr_f"])
        P.copy("dve", wr_b[:], wr_f[:], ["wr_f"], ["wr_b"])
        gT = psb("fgT", [128, 8]); P.ld(gT[:], gmem_d, "f1", [], ["gT"])
        gmoe = psb("gmoe", [128, D]); P.ld(gmoe[:], gmoe_d, "f2", [], ["gmoe"])
        brr = psb("brr", [128, 36]); P.ld(brr[:], br_d, "f3", [], ["brr"])
        slt_f = psb("slt_f", [128, 128]); slt_b = psb("slt_b", [128, 128], BF16)
        P.ld(slt_f[:], slt_d, "f4", [], ["slt_f"]); P.copy("dve", slt_b[:], slt_f[:], ["slt_f"], ["slt_b"])
        eoff = psb("eoff", [128, 32]); P.ld(eoff[:], eoff_d, "f5", [], ["eoff"])
        ones_b = psb("ones_b", [128, 128], BF16)
        P.op("dve", lambda e: e.memset(ones_b[:], 1.0), [], ["ones_b"])
        base = psb("base", [128, 32])
        P.op("dve", lambda e: e.memset(base[:], 0.0), [], ["base"])
        junk = psb("fjunk", [128, D], BF16); stat = psb("fstat", [128, 4]); xn_ = psb("fxn", [128, D], BF16)
        memt = psb("memt", [128, D]); memT = psb("memT", [128, 8, 256], BF16)
        mem_v = mem_d.rearrange("(n p) d -> n p d", p=128)
        for mt in range(2):
            P.ld(memt[:], mem_v[mt], "f6", [], ["memt"])
            norm_T(memt[:], "memt", gT, memT, "memT", slice(mt * 128, (mt + 1) * 128), (junk, stat, xn_), 7)
        KTm = psb("KTm", [128, 4, 256], BF16); Vm = psb("Vm", [128, 2, 512], BF16)
        for h in range(4):
            for k in range(8):
                P.mm(ps[0][:, 0:256], wckv_b[:, k, h * 128:(h + 1) * 128], memT[:, k, :], k == 0, k == 7, ["wckv_b", "memT"], ["ps0"])
            P.copy("dve", KTm[:, h, :], ps[0][:, 0:256], ["ps0"], ["KTm"])
        for mt in range(2):
            for k in range(8):
                P.mm(ps[1][:, :], memT[:, k, mt * 128:(mt + 1) * 128], wckv_b[:, k, 512:1024], k == 0, k == 7, ["wckv_b", "memT"], ["ps1"])
            P.copy("dve", Vm[:, mt, :], ps[1][:, :], ["ps1"], ["Vm"])
        h2T = [psb(f"h2T{i}", [128, 8, 512], BF16) for i in range(2)]
        qTh2 = [psb(f"qTh{i}", [128, 512], BF16) for i in range(2)]
        PT = [psb(f"fPT{i}", [128, 512], BF16) for i in range(2)]
        frec = psb("frec", [128, 512]); oT = psb("oT", [128, 4, 512], BF16)
        x1t = [psb(f"fx1t{i}", [128, D]) for i in range(4)]
        x2t = [psb(f"fx2t{i}", [128, D]) for i in range(4)]
        stat4 = psb("stat4", [128, 4, 4])
        h3g = [psb(f"h3g{i}", [128, 4, D], BF16) for i in range(2)]
        h3T = [psb(f"h3T{i}", [128, 8, 128], BF16) for i in range(2)]
        lg = psb("lg", [128, 144]); sm8 = psb("sm8", [128, 36]); gsel = psb("gsel", [128, 16]); pen = psb("pen", [128, 16])
        lem = psb("lem", [128, 128]); lem2 = psb("lem2", [128, 128]); oh1 = psb("oh1", [128, 128]); oh2 = psb("oh2", [128, 128])
        A_b = psb("A_b", [128, 128], BF16); rank = psb("rank", [128, 128]); tmp32 = psb("tmp32", [128, 128]); dst2 = psb("dst2", [128, 8])
        gej = psb("gej", [128, 16])
        X1v = X1.rearrange("(n p) d -> n p d", p=128)
        X2v = X2.rearrange("(n p) d -> n p d", p=128)
        pti = 0
        for g in range(8):
            s = g % 2
            osl = slice(g * 512, (g + 1) * 512)
            P.ld(h2T[s][:], H2T[:, :, osl].rearrange("c p t -> p c t"), f"h2T{s}", ["H2T"], [f"h2T{s}"])
            def f_qproj(h):
                qb = [0, 7][h % 2]
                for k in range(8):
                    P.mm(ps[qb][:, :], wcq_b[:, k, h * 128:(h + 1) * 128], h2T[s][:, k, :], k == 0, k == 7, ["wcq_b", f"h2T{s}"], [PSK[qb]])
                P.copy("dve", qTh2[h % 2][:], ps[qb][:, :], [PSK[qb]], [f"qTh{h % 2}"])

            f_qproj(0)
            for h in range(4):
                if h + 1 < 4:
                    f_qproj(h + 1)
                ob, db_ = ((3, 4), (5, 6))[h % 2]
                for mt in range(2):
                    b = 1 + mt
                    P.mm(ps[b][:, :], KTm[:, h, mt * 128:(mt + 1) * 128], qTh2[h % 2][:], True, True, ["KTm", f"qTh{h % 2}"], [PSK[b]])
                    p = pti % 2
                    pti += 1
                    P.act("act", PT[p][:], ps[b][:, :], AF.Exp, [PSK[b]], [f"fPT{p}"], scale=128.0 ** -0.5)
                    P.mm(ps[ob][:, :], Vm[:, mt, h * 128:(h + 1) * 128], PT[p][:], mt == 0, mt == 1, ["Vm", f"fPT{p}"], [PSK[ob]])
                    P.mm(ps[db_][:, :], ones_b[:], PT[p][:], mt == 0, mt == 1, ["ones_b", f"fPT{p}"], [PSK[db_]])
                P.op("dve", lambda e, db_=db_: e.reciprocal(frec[:], ps[db_][:, :]), [PSK[db_]], ["frec"])
                P.tt("dve", oT[:, h, :], ps[ob][:, :], frec[:], ALU.mult, [PSK[ob], "frec"], ["oT"])
            gs_ = g % 2
            h3k = f"h3g{gs_}"
            for t in range(4):
                i = g * 4 + t
                xs_ = t
                tcs = slice(t * 128, (t + 1) * 128)
                P.ld(x1t[xs_][:], X1v[i], f"fx1t{xs_}", ["X1"], [f"fx1t{xs_}"])
                for hf in range(2):
                    b = 5 + hf
                    for k in range(4):
                        P.mm(ps[b][:, :], oT[:, k, tcs], wco_b[:, k, hf * 512:(hf + 1) * 512], k == 0, k == 3, ["oT", "wco_b"], [PSK[b]])
                    P.tt("dve", x2t[xs_][:, hf * 512:(hf + 1) * 512], ps[b][:, :], x1t[xs_][:, hf * 512:(hf + 1) * 512], ALU.add,
                         [PSK[b], f"fx1t{xs_}"], [f"fx2t{xs_}"])
                P.st(X2v[i], x2t[xs_][:], f"fx2t{xs_}", [f"fx2t{xs_}"], ["X2"])
                xk = f"fx2t{xs_}"
                P.act("act", junk[:], x2t[xs_][:], AF.Square, [xk], ["junk", f"ns{t}a"], accum=stat4[:, t, 0:1])
                P.ts("dve", stat4[:, t, 1:2], stat4[:, t, 0:1], 1.0 / D, EPS, ALU.mult, ALU.add, [f"ns{t}a"], [f"ns{t}b"])
                P.act("act", stat4[:, t, 2:3], stat4[:, t, 1:2], AF.Ln, [f"ns{t}b"], [f"ns{t}c"])
                P.act("act", stat4[:, t, 3:4], stat4[:, t, 2:3], AF.Exp, [f"ns{t}c"], [f"ns{t}d"], scale=-0.5)
            def fB_tr(t):
                xs_ = t
                xk = f"fx2t{xs_}"
                hb = t % 2
                P.stt(h3g[gs_][:, t, :], x2t[xs_][:], stat4[:, t, 3:4], gmoe[:], ALU.mult, ALU.mult, [xk, f"ns{t}d", "gmoe"], [h3k])
                pt = ps[7][:].bitcast(BF16) if hb == 0 else ps[3][:].bitcast(BF16)
                pk = "ps7" if hb == 0 else "ps3"
                for k in range(8):
                    P.tr(pt[:, k * 128:(k + 1) * 128], h3g[gs_][:, t, k * 128:(k + 1) * 128], ident_b[:], [h3k, "ident_b"], [pk])
                P.copy(["act", "dve"][hb], h3T[hb][:].rearrange("p k t -> p (k t)"), pt, [pk], [f"h3T{hb}"])

            def fB_router(t):
                hb = t % 2
                for k in range(8):
                    P.mm(ps[0][:, t * 36:(t + 1) * 36], h3T[hb][:, k, :], wr_b[:, k, :], k == 0, k == 7, [f"h3T{hb}", "wr_b"], ["ps0"])

            fB_tr(0); fB_tr(1); fB_router(0); fB_tr(2); fB_router(1); fB_tr(3); fB_router(2); fB_router(3)
            i0 = g * 4
            gs_ = g % 2
            h3k = f"h3g{gs_}"
            lg3 = lg[:].rearrange("p (t n) -> p t n", t=4)
            P.tt("dve", lg3, ps[0][:, 0:144].rearrange("p (t n) -> p t n", t=4), bcast(brr[:].unsqueeze(1), [128, 4, 36]), ALU.add,
                 ["ps0", "brr"], ["lg"])
            lgg = lg3[:, :, 0:4]
            P.op("dve", lambda e, lgg=lgg: e.tensor_reduce(sm8[:, 0:4], lgg, AX.X, ALU.max), ["lg"], ["sm_a"])
            gsel3 = gsel[:].rearrange("p (t n) -> p t n", t=4)
            P.tt("dve", gsel3, lgg, bcast(sm8[:, 0:4].unsqueeze(2), [128, 4, 4]), ALU.is_ge, ["lg", "sm_a"], ["gsel"])
            gej3 = gej[:].rearrange("p (t n) -> p t n", t=4)
            P.tt("dve", gej3, lgg, bcast(sm8[:, 0:4].unsqueeze(2), [128, 4, 4]), ALU.subtract, ["lg", "sm_a"], ["gej"])
            P.act("act", gej[:], gej[:], AF.Exp, ["gej"], ["gej"])
            P.op("dve", lambda e, gej3=gej3: e.tensor_reduce(sm8[:, 4:8], gej3, AX.X, ALU.add), ["gej"], ["sm_c"])
            P.op("dve", lambda e: e.reciprocal(sm8[:, 8:12], sm8[:, 4:8]), ["sm_c"], ["sm_d"])
            P.ts("dve", pen[:], gsel[:], 1e30, -1e30, ALU.mult, ALU.add, ["gsel"], ["pen"])
            lem4 = lem[:].rearrange("p (t g e) -> p t g e", t=4, g=4)
            P.tt("dve", lem4, lg3[:, :, 4:36].rearrange("p t (g e) -> p t g e", g=4),
                 bcast(pen[:].rearrange("p (t g) -> p t g", t=4).unsqueeze(3), [128, 4, 4, 8]), ALU.add, ["lg", "pen"], ["lem"])
            lem3 = lem[:].rearrange("p (t n) -> p t n", t=4)
            lem23 = lem2[:].rearrange("p (t n) -> p t n", t=4)
            oh13 = oh1[:].rearrange("p (t n) -> p t n", t=4)
            oh23 = oh2[:].rearrange("p (t n) -> p t n", t=4)
            P.op("dve", lambda e, lem3=lem3: e.tensor_reduce(sm8[:, 12:16], lem3, AX.X, ALU.max), ["lem"], ["sm_m1"])
            P.tt("dve", oh13, lem3, bcast(sm8[:, 12:16].unsqueeze(2), [128, 4, 32]), ALU.is_equal, ["lem", "sm_m1"], ["oh1"])
            P.stt(lem2[:], oh1[:], -1e30, lem[:], ALU.mult, ALU.add, ["oh1", "lem"], ["lem2"])
            P.op("dve", lambda e, lem23=lem23: e.tensor_reduce(sm8[:, 16:20], lem23, AX.X, ALU.max), ["lem2"], ["sm_m2"])
            P.tt("dve", oh23, lem23, bcast(sm8[:, 16:20].unsqueeze(2), [128, 4, 32]), ALU.is_equal, ["lem2", "sm_m2"], ["oh2"])
            P.tt("dve", sm8[:, 20:24], sm8[:, 16:20], sm8[:, 12:16], ALU.subtract, ["sm_m1", "sm_m2"], ["sm_e"])
            P.act("act", sm8[:, 24:28], sm8[:, 20:24], AF.Exp, ["sm_e"], ["sm_f"])
            P.ts("dve", sm8[:, 28:32], sm8[:, 24:28], 1.0, None, ALU.add, None, ["sm_f"], ["sm_g"])
            P.op("dve", lambda e: e.reciprocal(sm8[:, 32:36], sm8[:, 28:32]), ["sm_g"], ["sm_h"])
            P.tt("dve", wall[:, i0:i0 + 4, 0], sm8[:, 8:12], sm8[:, 32:36], ALU.mult, ["sm_d", "sm_h"], ["wall"])
            P.tt("dve", wall[:, i0:i0 + 4, 1], wall[:, i0:i0 + 4, 0], sm8[:, 24:28], ALU.mult, ["wall", "sm_f"], ["wall"])
            A3 = A_b[:].rearrange("p (t n) -> p t n", t=4)
            P.tt("dve", A_b[:], oh1[:], oh2[:], ALU.add, ["oh1", "oh2"], ["A_b"])
            for t in range(4):
                P.mm(ps[1][:, t * 32:(t + 1) * 32], slt_b[:], A3[:, t, :], True, t == 0, ["slt_b", "A_b"], ["ps1"])
                for t2 in range(t):
                    P.mm(ps[1][:, t * 32:(t + 1) * 32], ones_b[:], A3[:, t2, :], False, t2 == t - 1, ["ones_b", "A_b"], ["ps1"])
            rank3 = rank[:].rearrange("p (t n) -> p t n", t=4)
            P.tt("dve", rank3, ps[1][:, 0:128].rearrange("p (t n) -> p t n", t=4), bcast(base[:].unsqueeze(1), [128, 4, 32]), ALU.add,
                 ["ps1", "base"], ["rank"])
            for t in range(4):
                P.mm(ps[2][:, 0:32], ones_b[:], A3[:, t, :], t == 0, t == 3, ["ones_b", "A_b"], ["ps2"])
            P.tt("dve", base[:], base[:], ps[2][:, 0:32], ALU.add, ["base", "ps2"], ["base"])
            P.ts("dve", rank[:], rank[:], float(CAP - 1), None, ALU.min, None, ["rank"], ["rank"])
            P.tt("dve", rank3, rank3, bcast(eoff[:].unsqueeze(1), [128, 4, 32]), ALU.add, ["rank", "eoff"], ["rank"])
            P.tt("dve", tmp32[:], rank[:], oh1[:], ALU.mult, ["rank", "oh1"], ["tmp32"])
            P.op("dve", lambda e: e.tensor_reduce(dst2[:, 0:4], tmp32[:].rearrange("p (t n) -> p t n", t=4), AX.X, ALU.add), ["tmp32"], ["dst2"])
            P.tt("dve", tmp32[:], rank[:], oh2[:], ALU.mult, ["rank", "oh2"], ["tmp32"])
            P.op("dve", lambda e: e.tensor_reduce(dst2[:, 4:8], tmp32[:].rearrange("p (t n) -> p t n", t=4), AX.X, ALU.add), ["tmp32"], ["dst2"])
            P.copy("dve", destall[:, i0:i0 + 4, :], dst2[:].rearrange("p (j t) -> p t j", j=2), ["dst2"], ["destall"])
            for t in range(4):
                for jj in range(2):
                    P.dma("pool", lambda e, i=i0 + t, jj=jj, t=t, gs_=gs_: e.indirect_dma_start(
                        out=XS[:, :], out_offset=bass.IndirectOffsetOnAxis(ap=destall[:, i, jj:jj + 1], axis=0),
                        in_=h3g[gs_][:, t, :], in_offset=None), f"sc{gs_}{t}{jj}", [h3k, "destall"], ["XS"])
        if dbg:
            DEST = nc.dram_tensor("DEST", [128, 64], I32, kind="ExternalOutput").ap()
            WALL = nc.dram_tensor("WALL", [128, 64], F32, kind="ExternalOutput").ap()
            P.st(DEST, destall[:].rearrange("p a b -> p (a b)"), "dd0", ["destall"], ["DEST"])
            P.st(WALL, wall[:].rearrange("p a b -> p (a b)"), "dd1", ["wall"], ["WALL"])
        P.barrier()
    if stage == "F":
        stop_here()
        return

    wg_d = dram_in("w_exp_gate", [NEXP, D, 512]); wu_d = dram_in("w_exp_up", [NEXP, D, 512]); wd_d = dram_in("w_exp_down", [NEXP, 512, D])
    YS = dram_scr("YS", [NROWS, D], F32)
    with ExitStack() as pes:
        def psb(name, shape, dt=F32):
            return pes.enter_context(nc.sbuf_tensor(uniq(name), list(shape), dt))
        wg_b = [psb(f"wg_b{i}", [128, 8, 512], BF16) for i in range(2)]
        wu_b = [psb(f"wu_b{i}", [128, 8, 512], BF16) for i in range(2)]
        wd_b = [psb(f"wd_b{i}", [128, 4, D], BF16) for i in range(2)]
        xs_t = [psb(f"xs_t{i}", [128, 3, D], BF16) for i in range(2)]
        xsT = [psb(f"xsT{i}", [128, 8, CAP], BF16) for i in range(2)]
        sgl = psb("sgl", [128, CAP]); hidT = psb("hidT", [128, 4, CAP], BF16)
        ysb = [psb(f"ysb{i}", [128, D]) for i in range(2)]
        ci = [0]
        yi = 0
        NBLK = CAP // 128
        def g_load_pieces(e_):
            s = e_ % 2
            pcs = []

            def castdma(dst, src, dkey):
                return lambda: P.dma("pool", lambda e: e.dma_start(dst, src), dkey + "q", [], [dkey])
            for half in range(2):
                pcs.append(castdma(wg_b[s][:, half * 4:(half + 1) * 4, :],
                           wg_d[e_, half * 512:(half + 1) * 512, :].rearrange("(k p) n -> p k n", p=128), f"wg_b{s}"))
                pcs.append(castdma(wu_b[s][:, half * 4:(half + 1) * 4, :],
                           wu_d[e_, half * 512:(half + 1) * 512, :].rearrange("(k p) n -> p k n", p=128), f"wu_b{s}"))
            for half in range(2):
                pcs.append(castdma(wd_b[s][:, half * 2:(half + 1) * 2, :],
                           wd_d[e_, half * 256:(half + 1) * 256, :].rearrange("(k p) n -> p k n", p=128), f"wd_b{s}"))
            pcs.append(lambda: P.ld(xs_t[s][:], XS[e_ * CAP:(e_ + 1) * CAP, :].rearrange("(t p) d -> p t d", p=128), f"xs_t{s}", ["XS"], [f"xs_t{s}"]))
            return pcs

        def g_transposes(e2, blocks):
            s2 = e2 % 2
            for t in blocks:
                pt = ps[6 + (t % 2)][:].bitcast(BF16)
                for k in range(8):
                    P.tr(pt[:, k * 128:(k + 1) * 128], xs_t[s2][:, t, k * 128:(k + 1) * 128], ident_b[:], [f"xs_t{s2}", "ident_b"], [PSK[6 + (t % 2)]])
                P.copy(["act", "dve"][t % 2], xsT[s2][:, :, t * 128:(t + 1) * 128], pt.rearrange("p (k c) -> p k c", k=8),
                       [PSK[6 + (t % 2)]], [f"xsT{s2}"])

        for pc in g_load_pieces(0):
            pc()
        g_transposes(0, range(NBLK))
        for e_ in range(NEXP):
            s = e_ % 2
            pieces = g_load_pieces(e_ + 1) if e_ + 1 < NEXP else []
            while pieces:
                pieces.pop()()
            for f in range(4):
                bg, bu = (2 * f) % 4, (2 * f + 1) % 4
                fsl = slice(f * 128, (f + 1) * 128)
                for k in range(8):
                    P.mm(ps[bg][:, 0:CAP], wg_b[s][:, k, fsl], xsT[s][:, k, :], k == 0, k == 7, [f"wg_b{s}", f"xsT{s}"], [PSK[bg]])
                for k in range(8):
                    P.mm(ps[bu][:, 0:CAP], wu_b[s][:, k, fsl], xsT[s][:, k, :], k == 0, k == 7, [f"wu_b{s}", f"xsT{s}"], [PSK[bu]])
                P.act("act", sgl[:], ps[bg][:, 0:CAP], AF.Silu, [PSK[bg]], ["sgl"])
                P.tt("dve", hidT[:, f, :], ps[bu][:, 0:CAP], sgl[:], ALU.mult, [PSK[bu], "sgl"], ["hidT"])
                if pieces:
                    pieces.pop(0)()
            for t in range(NBLK):
                ys = yi % 2
                yi += 1
                for hf in range(2):
                    b = (4, 5, 0, 1, 2, 3)[t * 2 + hf]
                    for f in range(4):
                        P.mm(ps[b][:, :], hidT[:, f, t * 128:(t + 1) * 128], wd_b[s][:, f, hf * 512:(hf + 1) * 512], f == 0, f == 3,
                             ["hidT", f"wd_b{s}"], [PSK[b]])
                    P.copy(["act", "dve"][hf], ysb[ys][:, hf * 512:(hf + 1) * 512], ps[b][:, :], [PSK[b]], [f"ysb{ys}"])
                r0 = e_ * CAP + t * 128
                P.st(YS[r0:r0 + 128, :], ysb[ys][:], f"ysb{ys}", [f"ysb{ys}"], ["YS"], queue="sp")
                if pieces:
                    pieces.pop(0)()
                if e_ + 1 < NEXP:
                    g_transposes(e_ + 1, [t])
        P.barrier()
    if stage == "G":
        stop_here()
        return

    gfin_d = dram_in("g_final_rep", [128, D])
    with ExitStack() as pes:
        def psb(name, shape, dt=F32):
            return pes.enter_context(nc.sbuf_tensor(uniq(name), list(shape), dt))
        gfin = psb("gfin", [128, D]); P.ld(gfin[:], gfin_d, "h0", [], ["gfin"])
        NS = 4
        y1 = [psb(f"y1_{i}", [128, D]) for i in range(NS)]
        y2 = [psb(f"y2_{i}", [128, D]) for i in range(NS)]
        x2t = [psb(f"hx2t{i}", [128, D]) for i in range(NS)]
        x3 = [psb(f"x3_{i}", [128, D]) for i in range(2)]
        ot = [psb(f"ot{i}", [128, D]) for i in range(2)]
        junk = psb("hjunk", [128, D], BF16); stat = [psb(f"hstat{i}", [128, 4]) for i in range(2)]
        X2v = X2.rearrange("(n p) d -> n p d", p=128)
        outv = out_d.rearrange("(n p) d -> n p d", p=128)
        def h_loads(i):
            s = i % NS
            P.ld(x2t[s][:], X2v[i], f"hx2t{s}", ["X2"], [f"hx2t{s}"])
            for jj, yb in enumerate((y1, y2)):
                P.dma("pool", lambda e, i=i, jj=jj, yb=yb, s=s: e.indirect_dma_start(
                    out=yb[s][:, :], out_offset=None, in_=YS[:, :],
                    in_offset=bass.IndirectOffsetOnAxis(ap=destall[:, i, jj:jj + 1], axis=0)),
                    f"ga{s}{jj}", ["YS", "destall"], [f"y{jj}_{s}"])

        def h_A(i):
            s = i % NS
            s2 = i % 2
            P.stt(x3[s2][:], y1[s][:], wall[:, i, 0:1], x2t[s][:], ALU.mult, ALU.add, [f"y0_{s}", "wall", f"hx2t{s}"], [f"x3_{s2}"])
            P.stt(x3[s2][:], y2[s][:], wall[:, i, 1:2], x3[s2][:], ALU.mult, ALU.add, [f"y1_{s}", "wall", f"x3_{s2}"], [f"x3_{s2}"])
            P.act("act", junk[:], x3[s2][:], AF.Square, [f"x3_{s2}"], ["junk", f"hs{s2}a"], accum=stat[s2][:, 0:1])

        def h_B(i):
            s2 = i % 2
            P.ts("dve", stat[s2][:, 1:2], stat[s2][:, 0:1], 1.0 / D, EPS, ALU.mult, ALU.add, [f"hs{s2}a"], [f"hs{s2}b"])
            P.act("act", stat[s2][:, 2:3], stat[s2][:, 1:2], AF.Ln, [f"hs{s2}b"], [f"hs{s2}c"])
            P.act("act", stat[s2][:, 3:4], stat[s2][:, 2:3], AF.Exp, [f"hs{s2}c"], [f"hs{s2}d"], scale=-0.5)
            P.stt(ot[s2][:], x3[s2][:], stat[s2][:, 3:4], gfin[:], ALU.mult, ALU.mult, [f"x3_{s2}", f"hs{s2}d", "gfin"], [f"ot{s2}"])
            P.st(outv[i], ot[s2][:], f"ot{s2}", [f"ot{s2}"], ["out"], is_out=True, queue="sp")

        for i in range(NS - 1):
            h_loads(i)
        h_A(0)
        for i in range(32):
            if i + NS - 1 < 32:
                h_loads(i + NS - 1)
            if i + 1 < 32:
                h_A(i + 1)
            h_B(i)
        P.barrier()
    P.finish()


DBG_OUT = {"QGT", "KGT", "AT", "QMT", "KMT", "ZGT", "ZMT", "VG", "KG", "RG", "VM", "OGT", "OMT", "X1", "H2T", "X2", "XS", "YS"}


def t5_bucket_np(dist):
    n = np.maximum(dist, 0)
    nf = np.maximum(n, 1).astype(np.float32)
    large = 16 + (np.log(nf / np.float32(16)) / np.float32(np.log(8.0)) * np.float32(16)).astype(np.int32)
    large = np.minimum(large, 31)
    return np.where(n < 16, n, large)


def host_consts(inputs):
    f = lambda k: np.asarray(inputs[k], np.float32)
    bf = ml_dtypes.bfloat16
    c = {}
    c["w_in"] = np.ascontiguousarray(f("w_in")[0])
    c["g_mix_t"] = np.ascontiguousarray(f("g_mix")[0].reshape(8, 128).T)
    c["g_cross_t"] = np.ascontiguousarray(f("g_cross")[0].reshape(8, 128).T)
    c["g_mem_t"] = np.ascontiguousarray(f("g_mem").reshape(8, 128).T)
    c["ident"] = np.eye(128, dtype=np.float32)
    c["w_alpha_up"] = np.ascontiguousarray(f("w_alpha_up")[0])
    c["b_alpha"] = np.ascontiguousarray(f("b_alpha")[0].reshape(1, 256))
    s_ = np.arange(128)
    c["u2"] = ((s_[:, None] <= s_[None, :]) & ((s_[:, None] // 64) == (s_[None, :] // 64))).astype(np.float32)
    c["u3"] = ((s_[:, None] > s_[None, :]) & ((s_[:, None] // 64) == (s_[None, :] // 64))).astype(np.float32)
    s6 = np.arange(64)
    c["maskT"] = (s6[:, None] <= s6[None, :]).astype(np.float32)
    c["ggla_rep"] = np.ascontiguousarray(np.broadcast_to(f("g_gla_head")[0].reshape(1, 512), (64, 512)))
    c["ind"] = (np.arange(32)[:, None] == (np.arange(TEXT)[None, :] // 256)).astype(bf)
    rb = f("rel_bias")
    j = np.arange(6)[:, None, None]; kl = np.arange(128)[None, :, None]; q = np.arange(512)[None, None, :]
    kr = j * 128 + kl - 256
    kb = np.floor_divide(kr, 256); qb = q // 256
    dist = q - kr
    bucket = t5_bucket_np(dist)
    tb = np.zeros((8, 6, 128, 512), np.float32)
    own = (kb == qb); prev = (kb == qb - 1)
    for h in range(8):
        g_ = rb[bucket, h]
        t = np.where(own, np.where(dist >= 0, g_, np.float32(NEG)), np.where(prev, g_, np.float32(0)))
        tb[h] = t
    c["tb"] = np.ascontiguousarray(tb.transpose(0, 2, 1, 3)).astype(bf)
    c["cfar_rep"] = np.ascontiguousarray(np.broadcast_to(rb[31].reshape(1, 8), (128, 8)))
    k_ = np.arange(128)[:, None]; m_ = np.arange(64)[None, :]
    c["sh"] = (k_ == m_ + 64).astype(np.float32)
    c["w_proj_gla"] = np.ascontiguousarray(f("w_proj_gla")[0]); c["w_proj_moba"] = np.ascontiguousarray(f("w_proj_moba")[0])
    c["w_out"] = np.ascontiguousarray(f("w_out")[0])
    c["w_cq"] = np.ascontiguousarray(f("w_cq")[0]); c["w_ckv"] = np.ascontiguousarray(f("w_ckv")[0]); c["w_co"] = np.ascontiguousarray(f("w_co")[0])
    c["g_moe_rep"] = np.ascontiguousarray(np.broadcast_to(f("g_moe")[0].reshape(1, D), (128, D)))
    c["w_router"] = np.ascontiguousarray(np.concatenate([f("w_router_group")[0], f("w_router_expert")[0]], axis=1))
    c["b_router_rep"] = np.ascontiguousarray(np.broadcast_to(
        np.concatenate([f("b_router_group")[0], f("b_router_expert")[0]]).reshape(1, 36), (128, 36)))
    c["slt"] = (s_[:, None] < s_[None, :]).astype(np.float32)
    c["eoff_rep"] = np.ascontiguousarray(np.broadcast_to((np.arange(32) * CAP).astype(np.float32).reshape(1, 32), (128, 32)))
    c["w_exp_gate"] = np.ascontiguousarray(f("w_exp_gate")[0]); c["w_exp_up"] = np.ascontiguousarray(f("w_exp_up")[0])
    c["w_exp_down"] = np.ascontiguousarray(f("w_exp_down")[0])
    c["g_final_rep"] = np.ascontiguousarray(np.broadcast_to(f("g_final").reshape(1, D), (128, D)))
    return c


def host_inputs(inputs, c, consts=None, names=None):
    if consts is None:
        consts = host_consts(inputs)
    b, half = c // 2, c % 2
    x = np.asarray(inputs["x"], np.float32)
    xe = np.zeros((TEXT, D), np.float32)
    if half == 1:
        xe[:] = x[b]
    else:
        xe[TOWN:] = x[b, :TOWN]
    m = dict(consts)
    m["x_ext"] = xe
    m["mem"] = np.ascontiguousarray(np.asarray(inputs["mem"], np.float32)[b])
    qt = np.arange(32)[:, None]; n = np.arange(32)[None, :]
    ownb = 16 + qt // 2
    valid = (n < ownb) & ((half == 1) | (n >= 16))
    rep = lambda a: np.ascontiguousarray(np.broadcast_to(a.astype(np.float32)[None], (128, 32, 32)))
    m["gadd"] = rep(np.where(valid, 0.0, -1e30))
    m["pv01"] = rep(valid)
    m["bfix"] = rep(np.where(n == ownb, 0.0, NEG))
    m["cfm"] = rep(n < ownb - 1)
    if names is not None:
        m = {k: v for k, v in m.items() if k in names}
    return m


def kernel(**inputs):
    nc = build("full")
    consts = host_consts(inputs)
    in_maps = [host_inputs(inputs, c, consts, set(DECL_INPUTS)) for c in range(NCORES)]
    res = run_bass_kernel_spmd(nc, in_maps, core_ids=list(range(NCORES)))
    out = np.zeros((4, 8192, D), np.float32)
    for c in range(NCORES):
        b, half = c // 2, c % 2
        out[b, half * TOWN:(half + 1) * TOWN] = res.results[c]["out"]
    return out
```
